# Optimizing a Trainium2 kernel written in Bass

```python
import jax, jax.numpy as jnp
from jax import lax
import numpy as np

D_MODEL = 2048
BATCH = 4
SEQ = 4096
DEPTH = 1

CHUNK = 64
FOX_HEADS = 8
FOX_HEAD_DIM = 128
FOX_WIDTH = FOX_HEADS * FOX_HEAD_DIM
QUERY_BLOCK = 128
RWKV_HEADS = 16
RWKV_HEAD_DIM = 64
RWKV_WIDTH = RWKV_HEADS * RWKV_HEAD_DIM
W_LORA = 96
A_LORA = 96
G_LORA = 256
D_MIX = FOX_WIDTH + RWKV_WIDTH
FOX_COLS = 4 * FOX_WIDTH + FOX_HEADS
RWKV_COLS = 3 * RWKV_WIDTH + W_LORA + A_LORA + G_LORA
IN_COLS = FOX_COLS + RWKV_COLS
PEER_HEADS = 8
PEER_NKEYS = 128
PEER_N_EXPERTS = PEER_NKEYS * PEER_NKEYS
PEER_DK = 256
PEER_DK_HALF = PEER_DK // 2
PEER_TOPK = 16
PEER_TOKEN_BLOCK = 128
NORM_EPS = 1e-6
GN_EPS = 64e-5

kernel_name = "hybrid_fox_rwkv7_peer_block"


def rms_norm(x, w):
    xf = x.astype(jnp.float32)
    y = xf * lax.rsqrt(jnp.mean(xf * xf, axis=-1, keepdims=True) + NORM_EPS)
    return (y * w.astype(jnp.float32)).astype(x.dtype)


def fox_mixer(cols, q_norm_w, k_norm_w, f_bias):
    B, S, _ = cols.shape
    q, k, v, gate = [cols[..., i * FOX_WIDTH:(i + 1) * FOX_WIDTH] for i in range(4)]
    f_logit = cols[..., 4 * FOX_WIDTH:] + f_bias
    heads = lambda t: t.reshape(B, S, FOX_HEADS, FOX_HEAD_DIM).transpose(0, 2, 1, 3)
    q = rms_norm(heads(q), q_norm_w)
    k = rms_norm(heads(k), k_norm_w)
    v = heads(v)
    log_f = jax.nn.log_sigmoid(f_logit.astype(jnp.float32)).transpose(0, 2, 1)
    cum = jnp.cumsum(log_f, axis=-1)
    scale = FOX_HEAD_DIM ** -0.5
    outs = []
    for i in range(S // QUERY_BLOCK):
        lo, hi = i * QUERY_BLOCK, (i + 1) * QUERY_BLOCK
        s = jnp.einsum('bhqd,bhkd->bhqk', q[:, :, lo:hi], k[:, :, :hi]).astype(jnp.float32) * scale
        s = s + (cum[:, :, lo:hi, None] - cum[:, :, None, :hi])
        causal = jnp.arange(lo, hi)[:, None] >= jnp.arange(hi)[None, :]
        p = jax.nn.softmax(jnp.where(causal, s, -jnp.inf), axis=-1)
        outs.append(jnp.einsum('bhqk,bhkd->bhqd', p.astype(v.dtype), v[:, :, :hi]))
    o = jnp.concatenate(outs, axis=2).transpose(0, 2, 1, 3).reshape(B, S, FOX_WIDTH)
    return o * jax.nn.sigmoid(gate)


def rwkv7_recurrence(r, w, k, v, a, b):
    B, S, H, N = r.shape
    to_chunks = lambda t: t.astype(jnp.float32).transpose(1, 0, 2, 3).reshape(S // CHUNK, CHUNK, B, H, N)

    def step(state, inp):
        r_t, w_t, k_t, v_t, a_t, b_t = inp
        sa = jnp.einsum('bhij,bhj->bhi', state, a_t)
        state = state * w_t[:, :, None, :] + sa[..., None] * b_t[:, :, None, :] + v_t[..., None] * k_t[:, :, None, :]
        return state, jnp.einsum('bhij,bhj->bhi', state, r_t)

    def chunk_step(state, chunk_inp):
        return lax.scan(step, state, chunk_inp)

    state0 = jnp.zeros((B, H, N, N), jnp.float32)
    _, y = lax.scan(chunk_step, state0, tuple(to_chunks(t) for t in (r, w, k, v, a, b)))
    return y.reshape(S, B, H, N).transpose(1, 0, 2, 3)


def rwkv7_mixer(cols, mu, w0, w_up, a0, a_up, g_up, k_k, k_a, r_k, ln_w, ln_b):
    B, S, _ = cols.shape
    prev = jnp.pad(cols, ((0, 0), (1, 0), (0, 0)))[:, :-1]
    cols = cols + (prev - cols) * mu
    W = RWKV_WIDTH
    r, k, v = cols[..., :W], cols[..., W:2 * W], cols[..., 2 * W:3 * W]
    wd = cols[..., 3 * W:3 * W + W_LORA]
    ad = cols[..., 3 * W + W_LORA:3 * W + W_LORA + A_LORA]
    gd = cols[..., 3 * W + W_LORA + A_LORA:]
    w_raw = -jax.nn.softplus(-(w0 + jnp.tanh(wd) @ w_up).astype(jnp.float32)) - 0.5
    decay = jnp.exp(-jnp.exp(w_raw))
    a = jax.nn.sigmoid((a0 + ad @ a_up).astype(jnp.float32))
    g = jax.nn.sigmoid(gd) @ g_up
    heads = lambda t: t.reshape(B, S, RWKV_HEADS, RWKV_HEAD_DIM)
    kk = heads((k * k_k).astype(jnp.float32))
    kk = kk / jnp.maximum(jnp.sqrt(jnp.sum(kk * kk, axis=-1, keepdims=True)), 1e-12)
    k = k * (1 + (a - 1) * k_a).astype(k.dtype)
    r_h, k_h, v_h, a_h = heads(r), heads(k), heads(v), heads(a)
    y = rwkv7_recurrence(r_h, heads(decay), k_h, v_h, -kk, kk * a_h)
    mean = jnp.mean(y, axis=-1, keepdims=True)
    var = jnp.mean(jnp.square(y - mean), axis=-1, keepdims=True)
    y = ((y - mean) * lax.rsqrt(var + GN_EPS)).reshape(B, S, W)
    y = (y * ln_w.astype(jnp.float32) + ln_b.astype(jnp.float32)).astype(cols.dtype)
    bonus = jnp.sum(r_h * k_h * r_k, axis=-1, keepdims=True) * v_h
    y = y + bonus.reshape(B, S, W)
    return y * g


def peer_route(h, w_query, sub_keys):
    T = h.shape[0]
    q = (h @ w_query).reshape(T, PEER_HEADS, 2, PEER_DK_HALF).astype(jnp.float32)
    s = jnp.einsum('thpd,hpkd->thpk', q, sub_keys.astype(jnp.float32))
    top_s, top_i = lax.top_k(s, PEER_TOPK)
    cand_s = (top_s[:, :, 0, :, None] + top_s[:, :, 1, None, :]).reshape(T, PEER_HEADS, PEER_TOPK * PEER_TOPK)
    cand_i = (top_i[:, :, 0, :, None] * PEER_NKEYS + top_i[:, :, 1, None, :]).reshape(T, PEER_HEADS, PEER_TOPK * PEER_TOPK)
    best_s, best_pos = lax.top_k(cand_s, PEER_TOPK)
    experts = jnp.take_along_axis(cand_i, best_pos, axis=-1)
    gates = jax.nn.softmax(best_s, axis=-1)
    return experts.reshape(T, PEER_HEADS * PEER_TOPK), gates.reshape(T, PEER_HEADS * PEER_TOPK)


def peer_experts(h, experts, gates, u, v):
    T, D = h.shape
    E = experts.shape[-1]
    nb = T // PEER_TOKEN_BLOCK

    def block(args):
        hb, eb, gb = args
        act = jax.nn.gelu(jnp.einsum('ted,td->te', u[eb], hb), approximate=False)
        return jnp.einsum('te,ted->td', (gb * act).astype(v.dtype), v[eb])

    out = lax.map(block, (h.reshape(nb, PEER_TOKEN_BLOCK, D),
                          experts.reshape(nb, PEER_TOKEN_BLOCK, E),
                          gates.reshape(nb, PEER_TOKEN_BLOCK, E)))
    return out.reshape(T, D)


def hybrid_layer(x, c, w_ada, b_ada, norm_mix_w, w_in, fox_q_norm_w, fox_k_norm_w, fox_f_bias,
                 rwkv_mu, rwkv_w0, rwkv_w_up, rwkv_a0, rwkv_a_up, rwkv_g_up, rwkv_k_k, rwkv_k_a,
                 rwkv_r_k, rwkv_ln_w, rwkv_ln_b, w_out, norm_ffn_w, peer_w_query, peer_sub_keys,
                 peer_u, peer_v):
    B, S, D = x.shape
    mod = jax.nn.silu(c) @ w_ada + b_ada
    sh1, sc1, g1, sh2, sc2, g2 = jnp.split(mod, 6, axis=-1)
    h = rms_norm(x, norm_mix_w) * (1 + sc1[:, None]) + sh1[:, None]
    proj = h @ w_in
    y_fox = fox_mixer(proj[..., :FOX_COLS], fox_q_norm_w, fox_k_norm_w, fox_f_bias)
    y_rwkv = rwkv7_mixer(proj[..., FOX_COLS:], rwkv_mu, rwkv_w0, rwkv_w_up, rwkv_a0, rwkv_a_up,
                         rwkv_g_up, rwkv_k_k, rwkv_k_a, rwkv_r_k, rwkv_ln_w, rwkv_ln_b)
    mix = jnp.concatenate([y_fox, y_rwkv], axis=-1) @ w_out
    x = x + g1[:, None] * mix
    h2 = rms_norm(x, norm_ffn_w) * (1 + sc2[:, None]) + sh2[:, None]
    tok = h2.reshape(B * S, D)
    experts, gates = peer_route(tok, peer_w_query, peer_sub_keys)
    ffn = peer_experts(tok, experts, gates, peer_u, peer_v).reshape(B, S, D)
    return x + g2[:, None] * ffn


def setup_inputs(seed: int = 0) -> dict:
    key = jax.random.key(seed)
    ks = jax.random.split(key, 28)
    n = lambda k, shape, s: jax.random.normal(k, shape, jnp.float32) * s
    L = DEPTH
    return {
        "x": n(ks[0], (BATCH, SEQ, D_MODEL), 1.0),
        "c": n(ks[1], (BATCH, D_MODEL), 1.0),
        "w_ada": n(ks[2], (L, D_MODEL, 6 * D_MODEL), D_MODEL ** -0.5),
        "b_ada": n(ks[3], (L, 6 * D_MODEL), 0.02),
        "norm_mix_w": 1.0 + n(ks[4], (L, D_MODEL), 0.02),
        "w_in": n(ks[5], (L, D_MODEL, IN_COLS), D_MODEL ** -0.5),
        "fox_q_norm_w": 1.0 + n(ks[6], (L, FOX_HEAD_DIM), 0.02),
        "fox_k_norm_w": 1.0 + n(ks[7], (L, FOX_HEAD_DIM), 0.02),
        "fox_f_bias": jax.random.uniform(ks[8], (L, FOX_HEADS), jnp.float32, 2.0, 5.0),
        "rwkv_mu": jax.random.uniform(ks[9], (L, RWKV_COLS), jnp.float32, 0.0, 1.0),
        "rwkv_w0": jax.random.uniform(ks[10], (L, RWKV_WIDTH), jnp.float32, -5.0, 1.0),
        "rwkv_w_up": n(ks[11], (L, W_LORA, RWKV_WIDTH), W_LORA ** -0.5),
        "rwkv_a0": n(ks[12], (L, RWKV_WIDTH), 0.1),
        "rwkv_a_up": n(ks[13], (L, A_LORA, RWKV_WIDTH), A_LORA ** -0.5),
        "rwkv_g_up": n(ks[14], (L, G_LORA, RWKV_WIDTH), G_LORA ** -0.5),
        "rwkv_k_k": 0.85 + n(ks[15], (L, RWKV_WIDTH), 0.05),
        "rwkv_k_a": 1.0 + n(ks[16], (L, RWKV_WIDTH), 0.05),
        "rwkv_r_k": n(ks[17], (L, RWKV_HEADS, RWKV_HEAD_DIM), 0.1),
        "rwkv_ln_w": 1.0 + n(ks[18], (L, RWKV_WIDTH), 0.02),
        "rwkv_ln_b": n(ks[19], (L, RWKV_WIDTH), 0.02),
        "w_out": n(ks[20], (L, D_MIX, D_MODEL), D_MIX ** -0.5),
        "norm_ffn_w": 1.0 + n(ks[21], (L, D_MODEL), 0.02),
        "peer_w_query": n(ks[22], (L, D_MODEL, PEER_HEADS * PEER_DK), D_MODEL ** -0.5),
        "peer_sub_keys": n(ks[23], (L, PEER_HEADS, 2, PEER_NKEYS, PEER_DK_HALF), PEER_DK_HALF ** -0.5),
        "peer_u": n(ks[24], (L, PEER_N_EXPERTS, D_MODEL), D_MODEL ** -0.5),
        "peer_v": n(ks[25], (L, PEER_N_EXPERTS, D_MODEL), PEER_HEADS ** -0.5),
    }


def reference(x, c, w_ada, b_ada, norm_mix_w, w_in, fox_q_norm_w, fox_k_norm_w, fox_f_bias,
              rwkv_mu, rwkv_w0, rwkv_w_up, rwkv_a0, rwkv_a_up, rwkv_g_up, rwkv_k_k, rwkv_k_a,
              rwkv_r_k, rwkv_ln_w, rwkv_ln_b, w_out, norm_ffn_w, peer_w_query, peer_sub_keys,
              peer_u, peer_v):
    for l in range(DEPTH):
        x = hybrid_layer(x, c, w_ada[l], b_ada[l], norm_mix_w[l], w_in[l], fox_q_norm_w[l],
                         fox_k_norm_w[l], fox_f_bias[l], rwkv_mu[l], rwkv_w0[l], rwkv_w_up[l],
                         rwkv_a0[l], rwkv_a_up[l], rwkv_g_up[l], rwkv_k_k[l], rwkv_k_a[l],
                         rwkv_r_k[l], rwkv_ln_w[l], rwkv_ln_b[l], w_out[l], norm_ffn_w[l],
                         peer_w_query[l], peer_sub_keys[l], peer_u[l], peer_v[l])
    return x
```

```python
import numpy as np
from contextlib import ExitStack
import concourse.bass as bass
import concourse.mybir as mybir
from concourse.bass_utils import run_bass_kernel_spmd

F32 = mybir.dt.float32
BF16 = mybir.dt.bfloat16
U32 = mybir.dt.uint32
F32R = mybir.dt.float32r
AF = mybir.ActivationFunctionType
ALU = mybir.AluOpType
AX = mybir.AxisListType

D = 2048
KD = 16
CTX = 4096
OWN = 2048
IN_COLS = 7624
RW0 = 4104
RWC = 3520
NE = 16384
NORM_EPS = 1e-6
GN_EPS = 64e-5


class Sched:
    EPOCH = 30000

    def __init__(self, nc, es, same_engine_sync=True, n_dma_sems=8):
        self.nc = nc
        self.es = es
        self.engs = {'pe': nc.tensor, 'act': nc.scalar, 'dve': nc.vector, 'pool': nc.gpsimd, 'sp': nc.sync}
        self.cur = {}
        self.nsem = 0
        for e in ('pe', 'act', 'dve', 'pool'):
            self._new_epoch(e)
        self.waited = {e: {} for e in self.engs}
        self.res = {}
        self.same = same_engine_sync
        self.dma = {}
        for q in ('sp', 'act', 'pool'):
            sems = [self._mk(f"dma_{q}_{i}") for i in range(n_dma_sems)]
            self.dma[q] = {'sems': sems, 'cnt': [0] * n_dma_sems, 'next': 0}
        self.n_inst = {e: 0 for e in self.engs}

    def _mk(self, name):
        self.nsem += 1
        return self.es.enter_context(self.nc.semaphore(name))

    def _new_epoch(self, e):
        self.cur[e] = [self._mk(f"c_{e}_{self.nsem}"), 0]

    def _deps(self, reads, writes):
        evs = []
        for k in reads:
            r = self.res.get(k)
            if r and r['w'] is not None:
                evs.append(r['w'])
        for k in writes:
            r = self.res.get(k)
            if r:
                if r['w'] is not None:
                    evs.append(r['w'] + ('waw',))
                evs.extend(r['r'].values())
        return evs

    def _commit(self, ev, reads, writes):
        for k in reads:
            r = self.res.setdefault(k, {'w': None, 'r': {}})
            old = r['r'].get(id(ev[0]))
            if old is None or old[1] < ev[1]:
                r['r'][id(ev[0])] = ev
        for k in writes:
            self.res[k] = {'w': ev, 'r': {}}

    def _wait(self, e, evs):
        eng = self.engs[e]
        best = {}
        for ev in evs:
            sem, val, prod = ev[0], ev[1], ev[2]
            if prod == e and (e == 'pe' or not self.same or len(ev) > 3):
                continue
            ev = (sem, val, prod)
            if self.waited[e].get(id(sem), 0) >= val:
                continue
            if id(sem) not in best or best[id(sem)][1] < val:
                best[id(sem)] = ev
        for sem, val, prod in best.values():
            eng.wait_ge(sem, val)
            self.waited[e][id(sem)] = val

    def op(self, e, fn, reads=(), writes=()):
        writes = list(writes) + [k for k in reads if k.startswith('pb')]
        reads = [k for k in reads if not k.startswith('pb')]
        self._wait(e, self._deps(reads, writes))
        c = self.cur[e]
        if c[1] >= self.EPOCH:
            self._new_epoch(e)
            c = self.cur[e]
        inst = fn()
        c[1] += 1
        inst.then_inc(c[0], 1)
        ev = (c[0], c[1], e)
        self._commit(ev, reads, writes)
        self.n_inst[e] += 1
        return inst

    def dma_op(self, q, out, in_, reads=(), writes=()):
        d = self.dma[q]
        i = d['next']
        d['next'] = (i + 1) % len(d['sems'])
        sem = d['sems'][i]
        evs = self._deps(reads, writes)
        if d['cnt'][i] > 0:
            evs.append((sem, d['cnt'][i], 'dma'))
        self._wait(q, evs)
        inst = self.engs[q].dma_start(out=out, in_=in_)
        d['cnt'][i] += 16
        inst.then_inc(sem, 16)
        ev = (sem, d['cnt'][i], 'dma')
        self._commit(ev, reads, writes)
        self.n_inst[q] += 1
        return inst

    def barrier(self):
        evs = []
        for r in self.res.values():
            if r['w'] is not None:
                evs.append(r['w'])
            evs.extend(r['r'].values())
        for e in ('pe', 'act', 'dve', 'pool', 'sp'):
            self._wait(e, [(s_, v_, 'x') for (s_, v_, p_) in evs])
        self.res = {k: v for k, v in self.res.items() if k.endswith('_d') or k == 'y'}

    def finish(self):
        evs = []
        for r in self.res.values():
            if r['w'] is not None:
                evs.append(r['w'])
            evs.extend(r['r'].values())
        self._wait('sp', evs)


INPUT_SPECS = {
    "xc": ([CTX, D], F32), "cT": ([128, KD], F32), "w_ada": ([D, 6 * D], F32), "b_adaT": ([128, 96], F32),
    "nw1T": ([128, KD], F32), "w_in": ([D, IN_COLS], F32), "qnw": ([128, 1], F32), "knw": ([128, 1], F32),
    "fbias": ([8], F32), "mu": ([RWC], F32), "w0": ([1024], F32), "w_up": ([96, 1024], F32), "a0": ([1024], F32),
    "a_up": ([96, 1024], F32), "g_up": ([256, 1024], F32), "k_k": ([1024], F32), "k_a": ([1024], F32),
    "r_k": ([1024], F32), "ln_w": ([1024], F32), "ln_b": ([1024], F32), "w_out": ([D, D], F32),
    "nw2T": ([128, KD], F32), "w_query": ([D, D], F32), "keysT": ([128, 16, 128], F32),
    "uT": ([128, 128, KD, 128], F32), "v": ([NE, D], F32),
    "ident": ([128, 128], F32), "tri": ([128, 128], F32), "ones": ([128, 128], F32), "e0sel": ([128, 128], F32),
    "tri2": ([128, 128], F32), "blk2": ([128, 128], F32), "cmask": ([64, 3, 512], F32), "idpat": ([128, 64], F32),
    "pre_mod": ([128, 96], F32), "pre_f": ([128, 32, 8], F32),
    "iota": ([128, 128], F32), "blockmask": ([128, 8], F32), "flagcol": ([128, 1], F32), "pflag": ([128, 1], F32),
}


def build(stages=("A", "B", "C", "D", "E", "F"), debug=()):
    nc = bass.Bass("TRN2", target_bir_lowering=False)
    class _Lazy(dict):
        def __missing__(self, n):
            sh, dt = INPUT_SPECS[n]
            self[n] = nc.dram_tensor(n, sh, dt, kind="ExternalInput").ap()
            return self[n]
    I = _Lazy()
    nc._used_inputs = I
    y = nc.dram_tensor("y", [OWN, D], F32, kind="ExternalOutput").ap()
    dbg = {}
    for n, sh, dt in debug:
        dbg[n] = nc.dram_tensor(n, sh, dt, kind="ExternalOutput").ap()
    qT_d = nc.dram_tensor("qT_d", [8, 128, OWN], BF16).ap()
    kT_d = nc.dram_tensor("kT_d", [8, 128, CTX], BF16).ap()
    sg_d = nc.dram_tensor("sg_d", [8, 128, OWN], BF16).ap()
    v_d = nc.dram_tensor("v_d", [CTX, 1024], BF16).ap()
    rw_d = nc.dram_tensor("rw_d", [CTX + 1, RWC], F32).ap()
    mixT_d = nc.dram_tensor("mixT_d", [KD, 128, OWN], BF16).ap()
    x1_d = nc.dram_tensor("x1_d", [OWN, D], F32).ap()
    h2T_d = nc.dram_tensor("h2T_d", [128, KD, OWN], BF16).ap()
    uTb_d = nc.dram_tensor("uTb_d", [128, 128, KD * 128], BF16).ap()
    vb_d = nc.dram_tensor("vb_d", [NE, D], BF16).ap()
    qp_d = nc.dram_tensor("qp_d", [128, 16, OWN], F32).ap()

    with ExitStack() as es:
        S = Sched(nc, es)
        T, V_, A_, P_ = nc.tensor, nc.vector, nc.scalar, nc.gpsimd

        def sb(ctx, name, shape, dt):
            return ctx.enter_context(nc.sbuf_tensor("sb_" + name, shape, dt))

        banks = [es.enter_context(nc.psum_tensor(f"pb{i}", [128, 512], F32)) for i in range(8)]
        pk = [f"pb{i}" for i in range(8)]

        def mm(out, lhsT, rhs, start, stop, r, w):
            return S.op('pe', lambda: T.matmul(out, lhsT, rhs, start=start, stop=stop), reads=r, writes=w)

        def tr(out, in_, ident, r, w):
            return S.op('pe', lambda: T.transpose(out, in_, ident), reads=r, writes=w)

        def act(out, in_, func, r, w, **kw):
            return S.op('act', lambda: A_.activation(out=out, in_=in_, func=func, **kw), reads=r, writes=w)

        def ts(e, out, in0, s1, s2, op0, op1, r, w):
            eng = V_ if e == 'dve' else P_
            if op1 is None:
                return S.op(e, lambda: eng.tensor_scalar(out=out, in0=in0, scalar1=s1, scalar2=None, op0=op0), reads=r, writes=w)
            return S.op(e, lambda: eng.tensor_scalar(out=out, in0=in0, scalar1=s1, scalar2=s2, op0=op0, op1=op1), reads=r, writes=w)

        def tt(e, out, in0, in1, op, r, w):
            eng = V_ if e == 'dve' else P_
            return S.op(e, lambda: eng.tensor_tensor(out=out, in0=in0, in1=in1, op=op), reads=r, writes=w)

        def stt(out, in0, scalar, in1, op0, op1, r, w):
            return S.op('dve', lambda: V_.scalar_tensor_tensor(out=out, in0=in0, scalar=scalar, in1=in1, op0=op0, op1=op1), reads=r, writes=w)

        def cp(e, out, in_, r, w):
            if e == 'act':
                return S.op('act', lambda: A_.copy(out=out, in_=in_), reads=r, writes=w)
            eng = V_ if e == 'dve' else P_
            return S.op(e, lambda: eng.tensor_copy(out=out, in_=in_), reads=r, writes=w)

        def dma(q, out, in_, r, w):
            return S.dma_op(q, out, in_, reads=r, writes=w)

        ident = sb(es, "ident", [128, 128], F32)
        identb = sb(es, "identb", [128, 128], BF16)
        onesb = sb(es, "onesb", [128, 128], BF16)
        ones = sb(es, "ones", [128, 128], F32)
        mod = sb(es, "mod", [128, 96], F32)
        f_sb = sb(es, "f_sb", [128, 32, 8], F32)
        flagcol = sb(es, "flagcol", [128, 1], F32)
        pflag = sb(es, "pflag", [128, 1], F32)
        dma('sp', ident[:], I["ident"], [], ['ident'])
        dma('sp', ones[:], I["ones"], [], ['ones'])
        dma('sp', flagcol[:], I["flagcol"], [], ['flagcol'])
        dma('sp', pflag[:], I["pflag"], [], ['pflag'])
        cp('dve', identb[:], ident[:], ['ident'], ['identb'])
        cp('dve', onesb[:], ones[:], ['ones'], ['onesb'])

        pending_copies = []
        if "F" in stages:
            uTv = I["uT"].rearrange("i p k j -> i p (k j)")
            for c in range(16):
                pending_copies.append((uTb_d[c * 8:(c + 1) * 8], uTv[c * 8:(c + 1) * 8], 'uTb_d'))
                pending_copies.append((vb_d[c * 1024:(c + 1) * 1024, :], I["v"][c * 1024:(c + 1) * 1024, :], 'vb_d'))

        def issue_copies(n):
            for _ in range(n):
                if pending_copies:
                    o_, i_, k_ = pending_copies.pop(0)
                    dma('pool', o_, i_, [], [k_])

        if "A" in stages:
            with ExitStack() as ph:
                cs = sb(ph, "cs", [128, KD], F32)
                sc = sb(ph, "sc", [128, KD], F32)
                bT = sb(ph, "bT", [128, 96], F32)
                wa = [sb(ph, f"wa{i}", [128, KD, 768], F32) for i in range(2)]
                dma('sp', cs[:], I["cT"], [], ['cs'])
                dma('sp', bT[:], I["b_adaT"], [], ['bT'])
                act(sc[:], cs[:], AF.Silu, ['cs'], ['sc'])
                wv = I["w_ada"].rearrange("(k p) c -> p k c", p=128)
                for g in range(16):
                    wt = wa[g % 2]
                    wk = f"wa{g % 2}"
                    dma('sp', wt[:, 0:8, :], wv[:, 0:8, g * 768:(g + 1) * 768], [], [wk])
                    dma('sp', wt[:, 8:16, :], wv[:, 8:16, g * 768:(g + 1) * 768], [], [wk])
                    for ct in range(6):
                        n = g * 6 + ct
                        for k in range(KD):
                            mm(banks[0][:, n:n + 1], wt[:, k, ct * 128:(ct + 1) * 128], sc[:, k:k + 1],
                               k == 0, k == KD - 1, [wk, 'sc'], [pk[0]])
                tt('dve', mod[:], banks[0][:, 0:96], bT[:], ALU.add, [pk[0], 'bT'], ['mod'])
        S.barrier()
        if "modT" in dbg:
            dma('sp', dbg["modT"], mod[:], ['mod'], ['dbg_modT'])

        if "A" not in stages:
            dma('sp', mod[:], I["pre_mod"], [], ['mod'])
        if "B" not in stages and "C" in stages:
            dma('sp', f_sb[:], I["pre_f"], [], ['f_sb'])
        sh1, sc1, g1c, sh2, sc2, g2c = [mod[:, i * 16:(i + 1) * 16] for i in range(6)]

        if "B" in stages:
            with ExitStack() as ph:
                hT = sb(ph, "hT", [128, KD, OWN], BF16)
                xt = [sb(ph, f"xt{i}", [128, D], F32) for i in range(2)]
                junk = sb(ph, "junk", [128, D], BF16)
                xn = sb(ph, "xn", [128, D], BF16)
                ss = sb(ph, "ss", [128, 32], F32)
                rstd = sb(ph, "rstd", [128, 32], F32)
                scl1 = sb(ph, "scl1", [128, KD], F32)
                nw1 = sb(ph, "nw1", [128, KD], F32)
                wg = [sb(ph, f"wg{i}", [128, KD, 512], BF16) for i in range(2)]
                wf = sb(ph, "wf", [128, KD, 8], BF16)
                stg = [sb(ph, f"stg{i}", [128, 512], F32) for i in range(2)]
                stgb = [sb(ph, f"stgb{i}", [128, 512], BF16) for i in range(2)]
                raw = sb(ph, "raw", [128, 512], F32)
                sq = sb(ph, "sq", [128, 512], BF16)
                lnt = sb(ph, "lnt", [128, 512], F32)
                rs = sb(ph, "rs", [128, 512], F32)
                qw = sb(ph, "qw", [128, 1], F32)
                kw_ = sb(ph, "kw_", [128, 1], F32)
                fb = sb(ph, "fb", [128, 8], F32)
                zrow = sb(ph, "zrow", [1, RWC], F32)
                epsc = sb(ph, "epsc", [128, 1], F32)
                S.op('dve', lambda: V_.memset(zrow[:], 0.0), [], ['zrow'])
                S.op('dve', lambda: V_.memset(epsc[:], NORM_EPS), [], ['epsc'])
                dma('sp', rw_d[0:1, :], zrow[:], ['zrow'], ['rw_d'])
                dma('sp', nw1[:], I["nw1T"], [], ['nw1'])
                dma('sp', qw[:], I["qnw"], [], ['qw'])
                dma('sp', kw_[:], I["knw"], [], ['kw_'])
                dma('sp', fb[:], I["fbias"].partition_broadcast(128), [], ['fb'])
                stt(scl1[:], sc1, 1.0, nw1[:], ALU.add, ALU.mult, ['mod', 'nw1'], ['scl1'])
                ts('dve', qw[:], qw[:], 128.0 ** -0.5, None, ALU.mult, None, ['qw'], ['qw'])
                wv = I["w_in"].rearrange("(k p) c -> p k c", p=128)
                dma('pool', wf[:], wv[:, :, 4096:4104], [], ['wf'])
                gi = 0
                obank = 0
                for th in range(2):
                    for t16 in range(16):
                        tile = th * 16 + t16
                        xb_ = xt[tile % 2]
                        xk = f"xt{tile % 2}"
                        dma('sp', xb_[:], I["xc"][tile * 128:(tile + 1) * 128, :], [], [xk])
                        act(junk[:], xb_[:], AF.Square, [xk], ['junk'], accum_out=ss[:, tile:tile + 1])
                        ts('dve', rstd[:, tile:tile + 1], ss[:, tile:tile + 1], 1.0 / D, NORM_EPS, ALU.mult, ALU.add, ['junk'], ['rstd'])
                        act(rstd[:, tile:tile + 1], rstd[:, tile:tile + 1], AF.Sqrt, ['rstd'], ['rstd'])
                        S.op('dve', lambda: V_.reciprocal(out=rstd[:, tile:tile + 1], in_=rstd[:, tile:tile + 1]), ['rstd'], ['rstd'])
                        act(xn[:], xb_[:], AF.Copy, [xk, 'rstd'], ['xn'], scale=rstd[:, tile:tile + 1])
                        for half in range(2):
                            pbv = banks[half][:].bitcast(BF16)
                            for kk in range(8):
                                k = half * 8 + kk
                                tr(pbv[:, kk * 128:(kk + 1) * 128], xn[:, k * 128:(k + 1) * 128], identb[:], ['xn', 'identb'], [pk[half]])
                            for kk in range(8):
                                k = half * 8 + kk
                                o = hT[:, k, t16 * 128:(t16 + 1) * 128]
                                if kk % 2 == 0:
                                    ts('dve', o, pbv[:, kk * 128:(kk + 1) * 128], scl1[:, k:k + 1], sh1[:, k:k + 1], ALU.mult, ALU.add,
                                       [pk[half], 'scl1', 'mod'], ['hT'])
                                else:
                                    act(o, pbv[:, kk * 128:(kk + 1) * 128], AF.Identity, [pk[half], 'scl1', 'mod'], ['hT'],
                                        scale=scl1[:, k:k + 1], bias=sh1[:, k:k + 1])
                    groups = []
                    if th == 1:
                        groups += [("q", 0, 512), ("q", 512, 512)]
                    groups += [("k", 1024, 512), ("k", 1536, 512), ("v", 2048, 512), ("v", 2560, 512)]
                    if th == 1:
                        groups += [("g", 3072, 512), ("g", 3584, 512)]
                    c0 = RW0
                    while c0 < IN_COLS:
                        n = min(512, IN_COLS - c0)
                        groups.append(("r", c0, n))
                        c0 += n
                    for (kind, c0, n) in groups:
                        wt = wg[gi % 2]
                        wk = f"wg{gi % 2}"
                        gi += 1
                        dma('pool', wt[:, :, 0:n], wv[:, :, c0:c0 + n], [], [wk])
                        issue_copies(2)
                        if kind in ("q", "k", "g"):
                            for tb in range(4):
                                for hh in range(4):
                                    head = (c0 % 1024) // 128 + hh
                                    bk = 2 + obank % 4
                                    obank += 1
                                    for k in range(KD):
                                        mm(banks[bk][:], wt[:, k, hh * 128:(hh + 1) * 128], hT[:, k, tb * 512:(tb + 1) * 512],
                                           k == 0, k == KD - 1, [wk, 'hT'], [pk[bk]])
                                    so = stgb[obank % 2]
                                    sk = f"stgb{obank % 2}"
                                    if kind == "g":
                                        act(so[:], banks[bk][:], AF.Sigmoid, [pk[bk]], [sk])
                                        dma('sp', sg_d[head, :, tb * 512:(tb + 1) * 512], so[:], [sk], ['sg_d'])
                                    else:
                                        cp('dve', raw[:], banks[bk][:], [pk[bk]], ['raw'])
                                        act(sq[:], banks[bk][:], AF.Square, [pk[bk]], ['sq'])
                                        mm(banks[6][:], onesb[:], sq[:], True, True, ['onesb', 'sq'], [pk[6]])
                                        act(lnt[:], banks[6][:], AF.Ln, [pk[6], 'epsc'], ['lnt'], scale=1.0 / 128, bias=epsc[:])
                                        act(rs[:], lnt[:], AF.Exp, ['lnt'], ['rs'], scale=-0.5)
                                        wcol = qw if kind == "q" else kw_
                                        stt(so[:], raw[:], wcol[:, 0:1], rs[:], ALU.mult, ALU.mult, ['raw', 'rs', 'qw', 'kw_'], [sk])
                                        if kind == "q":
                                            dma('sp', qT_d[head, :, tb * 512:(tb + 1) * 512], so[:], [sk], ['qT_d'])
                                        else:
                                            dma('sp', kT_d[head, :, th * OWN + tb * 512: th * OWN + (tb + 1) * 512], so[:], [sk], ['kT_d'])
                        else:
                            for t16 in range(16):
                                tile = th * 16 + t16
                                bk = 2 + obank % 4
                                obank += 1
                                for k in range(KD):
                                    mm(banks[bk][:, 0:n], hT[:, k, t16 * 128:(t16 + 1) * 128], wt[:, k, 0:n],
                                       k == 0, k == KD - 1, [wk, 'hT'], [pk[bk]])
                                if kind == "v":
                                    so = stgb[obank % 2]
                                    sk = f"stgb{obank % 2}"
                                    if obank % 2:
                                        act(so[:, 0:n], banks[bk][:, 0:n], AF.Copy, [pk[bk]], [sk])
                                    else:
                                        cp('dve', so[:, 0:n], banks[bk][:, 0:n], [pk[bk]], [sk])
                                    dma('sp', v_d[tile * 128:(tile + 1) * 128, c0 - 2048:c0 - 2048 + n], so[:, 0:n], [sk], ['v_d'])
                                else:
                                    so = stg[obank % 2]
                                    sk = f"stg{obank % 2}"
                                    if obank % 2:
                                        act(so[:, 0:n], banks[bk][:, 0:n], AF.Copy, [pk[bk]], [sk])
                                    else:
                                        cp('dve', so[:, 0:n], banks[bk][:, 0:n], [pk[bk]], [sk])
                                    dma('sp', rw_d[1 + tile * 128:1 + (tile + 1) * 128, c0 - RW0:c0 - RW0 + n], so[:, 0:n], [sk], ['rw_d'])
                    for t16 in range(16):
                        tile = th * 16 + t16
                        for k in range(KD):
                            mm(banks[7][:, 0:8], hT[:, k, t16 * 128:(t16 + 1) * 128], wf[:, k, :], k == 0, k == KD - 1, ['wf', 'hT'], [pk[7]])
                        tt('dve', f_sb[:, tile, :], banks[7][:, 0:8], fb[:], ALU.add, [pk[7], 'fb'], ['f_sb'])
        S.barrier()
        for n in ("qT_d", "kT_d", "sg_d", "v_d", "rw_d"):
            if "dbg_" + n in dbg:
                src = {"qT_d": qT_d, "kT_d": kT_d, "sg_d": sg_d, "v_d": v_d, "rw_d": rw_d}[n]
                dma('sp', dbg["dbg_" + n], src, [n], ["dbg_" + n])
        if "dbg_f" in dbg:
            dma('sp', dbg["dbg_f"], f_sb[:], ['f_sb'], ['dbg_f'])

        issue_copies(len(pending_copies))
        if "C" in stages:
            with ExitStack() as ph:
                tri = sb(ph, "tri", [128, 128], F32)
                trib = sb(ph, "trib", [128, 128], BF16)
                e0 = sb(ph, "e0", [128, 128], F32)
                lf = sb(ph, "lf", [128, 256], F32)
                cum = sb(ph, "cum", [128, 256], F32)
                tot = sb(ph, "tot", [128, 256], F32)
                off = sb(ph, "off", [128, 256], F32)
                ncb = sb(ph, "ncb", [128, 256], F32)
                cref = sb(ph, "cref", [128, 256], F32)
                kbcol = sb(ph, "kbcol", [128, 1], F32)
                onec = sb(ph, "onec", [128, 1], F32)
                biasall = sb(ph, "biasall", [128, 8, 16, 32], F32)
                kT_h = sb(ph, "kT_h", [128, CTX], BF16)
                qT_h = sb(ph, "qT_h", [128, OWN], BF16)
                v_h = sb(ph, "v_h", [128, 32, 128], BF16)
                sg_h = sb(ph, "sg_h", [128, OWN], BF16)
                pT = [sb(ph, f"pT{i}", [128, 512], BF16) for i in range(2)]
                rec = sb(ph, "rec", [128, 512], F32)
                o1 = sb(ph, "o1", [128, 512], F32)
                o2 = [sb(ph, f"o2{i}", [128, 512], BF16) for i in range(2)]
                dma('sp', tri[:], I["tri"], [], ['tri'])
                dma('sp', e0[:], I["e0sel"], [], ['e0'])
                cp('dve', trib[:], tri[:], ['tri'], ['trib'])
                S.op('dve', lambda: V_.memset(onec[:], 1.0), [], ['onec'])
                fflat = f_sb[:].rearrange("p c h -> p (c h)")
                act(lf[:], fflat, AF.Exp, ['f_sb'], ['lf'], scale=-1.0)
                act(lf[:], lf[:], AF.Ln, ['lf', 'onec'], ['lf'], bias=onec[:])
                ts('dve', lf[:], lf[:], -1.0, None, ALU.mult, None, ['lf'], ['lf'])
                mm(banks[0][:, 0:256], tri[:], lf[:], True, True, ['tri', 'lf'], [pk[0]])
                mm(banks[1][:, 0:256], ones[:], lf[:], True, True, ['ones', 'lf'], [pk[1]])
                cp('dve', tot[:], banks[1][:, 0:256], [pk[1]], ['tot'])
                S.op('dve', lambda: V_.memset(off[:], 0.0), [], ['off'])
                for c in range(1, 32):
                    tt('dve', off[:, c * 8:(c + 1) * 8], off[:, (c - 1) * 8:c * 8], tot[:, (c - 1) * 8:c * 8], ALU.add, ['off', 'tot'], ['off'])
                tt('dve', cum[:], banks[0][:, 0:256], off[:], ALU.add, [pk[0], 'off'], ['cum'])
                mm(banks[2][:, 0:256], e0[:], cum[:], True, True, ['e0', 'cum'], [pk[2]])
                cp('dve', cref[:], banks[2][:, 0:256], [pk[2]], ['cref'])
                ts('dve', kbcol[:], flagcol[:], -1.0, 30000.0, ALU.add, ALU.mult, ['flagcol'], ['kbcol'])
                ts('dve', ncb[:], cum[:], -1.0, None, ALU.mult, None, ['cum'], ['ncb'])
                ts('dve', ncb[:, 0:128], ncb[:, 0:128], kbcol[:, 0:1], None, ALU.add, None, ['ncb', 'kbcol'], ['ncb'])
                ncb3 = ncb[:].rearrange("p (c h) -> p c h", h=8)
                for h in range(8):
                    for qi in range(16):
                        col = (16 + qi) * 8 + h
                        ts('dve', biasall[:, h, qi, :], ncb3[:, :, h], cref[:, col:col + 1], None, ALU.add, None, ['ncb', 'cref'], ['biasall'])
                vview = v_d.rearrange("(c p) f -> p c f", p=128)
                ob = 0
                for h in range(8):
                    dma('sp', kT_h[:], kT_d[h], ['kT_d'], ['kT_h'])
                    dma('sp', qT_h[:], qT_d[h], ['qT_d'], ['qT_h'])
                    dma('sp', v_h[:], vview[:, :, h * 128:(h + 1) * 128], ['v_d'], ['v_h'])
                    dma('sp', sg_h[:], sg_d[h], ['sg_d'], ['sg_h'])
                    for qb in range(4):
                        nk = 16 + 4 * qb + 4
                        bO = banks[2 + 2 * (ob % 2)]
                        kO = pk[2 + 2 * (ob % 2)]
                        bR = banks[3 + 2 * (ob % 2)]
                        kR = pk[3 + 2 * (ob % 2)]
                        def fox_qk(kt):
                            c0 = max(0, kt - (16 + 4 * qb)) * 128
                            mm(banks[kt % 2][:, c0:512], kT_h[:, kt * 128:(kt + 1) * 128], qT_h[:, qb * 512 + c0:(qb + 1) * 512], True, True,
                               ['kT_h', 'qT_h'], [pk[kt % 2]])

                        def fox_act(kt):
                            jmin = max(0, kt - (16 + 4 * qb))
                            sbk = banks[kt % 2]
                            sk = pk[kt % 2]
                            pt = pT[kt % 2]
                            ptk = f"pT{kt % 2}"
                            for j in range(jmin, 4):
                                qi = 4 * qb + j
                                act(pt[:, j * 128:(j + 1) * 128], sbk[:, j * 128:(j + 1) * 128], AF.Exp, [sk, 'biasall'], [ptk],
                                    bias=biasall[:, h, qi, kt:kt + 1], scale=1.0)
                                if kt == 16 + qi:
                                    tt('dve', pt[:, j * 128:(j + 1) * 128], pt[:, j * 128:(j + 1) * 128], trib[:], ALU.mult, [ptk, 'trib'], [ptk])

                        def fox_pv(kt):
                            c0 = max(0, kt - (16 + 4 * qb)) * 128
                            pt = pT[kt % 2]
                            ptk = f"pT{kt % 2}"
                            mm(bO[:, c0:512], v_h[:, kt, :], pt[:, c0:512], kt == 0, kt == nk - 1, ['v_h', ptk], [kO])
                            mm(bR[:, c0:512], onesb[:], pt[:, c0:512], kt == 0, kt == nk - 1, ['onesb', ptk], [kR])

                        fox_qk(0)
                        for kt in range(nk):
                            fox_act(kt)
                            if kt + 1 < nk:
                                fox_qk(kt + 1)
                            fox_pv(kt)
                        S.op('dve', lambda: V_.reciprocal(out=rec[:], in_=bR[:]), [kR], ['rec'])
                        tt('dve', o1[:], bO[:], rec[:], ALU.mult, [kO, 'rec'], ['o1'])
                        oo = o2[ob % 2]
                        ok_ = f"o2{ob % 2}"
                        tt('pool', oo[:], o1[:], sg_h[:, qb * 512:(qb + 1) * 512], ALU.mult, ['o1', 'sg_h'], [ok_])
                        dma('sp', mixT_d[h, :, qb * 512:(qb + 1) * 512], oo[:], [ok_], ['mixT_d'])
                        ob += 1
            S.barrier()
        if "D" in stages:
            with ExitStack() as ph:
                mub = sb(ph, "mub", [128, RWC], F32)
                cvec = {}
                for nm in ("w0", "a0", "k_k", "k_a", "r_k", "ln_w", "ln_b"):
                    cvec[nm] = sb(ph, "c_" + nm, [128, 1024], F32)
                    dma('sp', cvec[nm][:], I[nm].partition_broadcast(128), [], ['c_' + nm])
                dma('sp', mub[:], I["mu"].partition_broadcast(128), [], ['mub'])
                wup = sb(ph, "wup", [96, 1024], F32)
                aup = sb(ph, "aup", [96, 1024], F32)
                gup = sb(ph, "gup", [128, 2, 1024], F32)
                dma('sp', wup[:], I["w_up"], [], ['wup'])
                dma('sp', aup[:], I["a_up"], [], ['aup'])
                dma('sp', gup[:], I["g_up"].rearrange("(k p) c -> p k c", p=128), [], ['gup'])
                tri2 = sb(ph, "tri2", [128, 128], F32)
                blk2 = sb(ph, "blk2", [128, 128], F32)
                idp = sb(ph, "idp", [128, 64], F32)
                cmf = sb(ph, "cmf", [64, 3, 512], F32)
                cmb = sb(ph, "cmb", [64, 3, 512], BF16)
                irep = sb(ph, "irep", [64, 8, 64], BF16)
                dma('sp', tri2[:], I["tri2"], [], ['tri2'])
                dma('sp', blk2[:], I["blk2"], [], ['blk2'])
                dma('sp', idp[:], I["idpat"], [], ['idp'])
                dma('sp', cmf[:], I["cmask"], [], ['cmf'])
                cp('dve', cmb[:], cmf[:], ['cmf'], ['cmb'])
                cp('dve', irep[:], idp[0:64, :].unsqueeze(1).to_broadcast([64, 8, 64]), ['idp'], ['irep'])
                MU_S, MU_I, ML_S = cmb[:, 0, :], cmb[:, 1, :], cmb[:, 2, :]
                i64b = identb[0:64, 0:64]
                Rb = sb(ph, "Rb", [128, 1024], F32)
                Kb = sb(ph, "Kb", [128, 1024], F32)
                Vb = sb(ph, "Vb", [128, 1024], F32)
                LO = sb(ph, "LO", [128, 448], F32)
                A1 = sb(ph, "A1", [128, 1024], F32)
                A2 = sb(ph, "A2", [128, 1024], F32)
                A3 = sb(ph, "A3", [128, 1024], F32)
                A4 = sb(ph, "A4", [128, 1024], F32)
                A5 = sb(ph, "A5", [128, 1024], F32)
                A6 = sb(ph, "A6", [128, 1024], F32)
                TMP = sb(ph, "TMP", [128, 1024], F32)
                st16 = sb(ph, "st16", [128, 16], F32)
                st16b = sb(ph, "st16b", [128, 16], F32)
                lt = sb(ph, "lt", [128, 448], F32)
                ltT = sb(ph, "ltT", [128, 4, 128], F32)
                tm = {}
                for nm in ("rh", "ah", "bh", "kh", "bb", "kb", "vb", "Fm"):
                    tm[nm] = sb(ph, "t_" + nm, [128, 1024], BF16)
                sh = {}
                for nm in ("ah", "vb", "bb", "kb", "Fm"):
                    sh[nm] = sb(ph, "s_" + nm, [64, 1024], BF16)
                FM = sb(ph, "FM", [64, 16, 4, 128], BF16)
                irepf = sb(ph, "irepf", [64, 8, 64], F32)
                cp('dve', irepf[:], idp[0:64, :].unsqueeze(1).to_broadcast([64, 8, 64]), ['idp'], ['irepf'])
                onesw = sb(ph, "onesw", [64, 512], F32)
                identr = sb(ph, "identr", [64, 64], F32)
                S.op('dve', lambda: V_.memset(onesw[:], 1.0), [], ['onesw'])
                tt('dve', identr[:].bitcast(F32R), ident[0:64, 0:64], onesw[:, 0:64], ALU.mult, ['ident', 'onesw'], ['identr'])
                CB = [{}, {}]
                CB[0]['Lb'] = [(sb(ph, f"Lb{i}", [64, 8, 64], F32)[:], f"Lb{i}") for i in range(2)]
                CB[0]['Lpb'] = [(sb(ph, f"Lpb{i}", [64, 8, 64], F32)[:], f"Lpb{i}") for i in range(2)]
                CB[0]['TTb'] = [(sb(ph, f"TTb{i}", [64, 8, 64], F32)[:], f"TTb{i}") for i in range(2)]
                for nm in ('TTfb', 'MakT', 'MrbT', 'MrkT', 'Wb', 'Atb', 'Vtb', 'RtT', 'Pmb'):
                    CB[0][nm] = (sb(ph, nm, [64, 8, 64], BF16)[:], nm)

                def v32(buf, half):
                    return buf[0:64, half * 512:(half + 1) * 512].rearrange("p (h c) -> p h c", c=64)

                def v16(buf, q):
                    return buf[:].bitcast(BF16)[0:64, q * 512:(q + 1) * 512].rearrange("p (h c) -> p h c", c=64)

                CB[1]['Lb'] = [(sb(ph, f"Lb1_{i}", [64, 8, 64], F32)[:], f"Lb1_{i}") for i in range(2)]
                CB[1]['Lpb'] = [(sb(ph, f"Lpb1_{i}", [64, 8, 64], F32)[:], f"Lpb1_{i}") for i in range(2)]
                CB[1]['TTb'] = [(sb(ph, f"TTb1_{i}", [64, 8, 64], F32)[:], f"TTb1_{i}") for i in range(2)]
                for q_, nm in enumerate(('TTfb', 'MakT', 'MrbT', 'MrkT')):
                    CB[1][nm] = (v16(A6, q_), 'A6')
                for q_, nm in enumerate(('Wb', 'Atb', 'Vtb', 'RtT')):
                    CB[1][nm] = (v16(Kb, q_), 'Kb')
                CB[1]['Pmb'] = (sb(ph, "Pmb1", [64, 8, 64], BF16)[:], 'Pmb1')
                Zb = sb(ph, "Zb", [64, 16, 64], BF16)
                Yt = sb(ph, "Yt", [128, 1024], F32)
                PRV = Yt
                Ych = TMP[0:64, :]
                yb = sb(ph, "yb", [128, 1024], BF16)
                mst = sb(ph, "mst", [128, 8, 128], BF16)
                S.op('dve', lambda: V_.memset(Zb[:], 0.0), [], ['Zb'])
                bank_ctr = [0]

                def nbk():
                    b = bank_ctr[0] % 8
                    bank_ctr[0] += 1
                    return b

                def h3(t_, w=64):
                    return t_.rearrange("p (h c) -> p h c", c=w)

                def bc16(t16_):
                    return t16_.unsqueeze(2).to_broadcast([128, 16, 64])

                for tile in range(32):
                    own = tile >= 16
                    r0 = tile * 128
                    for si_, (buf, key, c0, n) in enumerate(((Rb, 'Rb', 0, 1024), (Kb, 'Kb', 1024, 1024), (Vb, 'Vb', 2048, 1024), (LO, 'LO', 3072, 448))):
                        pv, pvk = (Yt, 'Yt') if si_ % 2 == 0 else (TMP, 'TMP')
                        dma('sp', buf[:, 0:n], rw_d[1 + r0:1 + r0 + 128, c0:c0 + n], ['rw_d'], [key])
                        dma('sp', pv[:, 0:n], rw_d[r0:r0 + 128, c0:c0 + n], ['rw_d'], [pvk])
                        if tile == 16:
                            ts('dve', pv[:, 0:n], pv[:, 0:n], pflag[:, 0:1], None, ALU.mult, None, [pvk, 'pflag'], [pvk])
                        tt('pool', pv[:, 0:n], pv[:, 0:n], buf[:, 0:n], ALU.subtract, [pvk, key], [pvk])
                        tt('pool', pv[:, 0:n], pv[:, 0:n], mub[:, c0:c0 + n], ALU.mult, [pvk, 'mub'], [pvk])
                        tt('dve', buf[:, 0:n], buf[:, 0:n], pv[:, 0:n], ALU.add, [key, pvk], [key])
                    act(lt[:, 0:96], LO[:, 0:96], AF.Tanh, ['LO'], ['lt'])
                    cp('dve', lt[:, 96:192], LO[:, 96:192], ['LO'], ['lt'])
                    act(lt[:, 192:448], LO[:, 192:448], AF.Sigmoid, ['LO'], ['lt'])
                    bT_ = nbk()
                    tr(banks[bT_][0:96, 0:128], lt[:, 0:96], ident[:], ['lt', 'ident'], [pk[bT_]])
                    tr(banks[bT_][0:96, 128:256], lt[:, 96:192], ident[:], ['lt', 'ident'], [pk[bT_]])
                    tr(banks[bT_][:, 256:384], lt[:, 192:320], ident[:], ['lt', 'ident'], [pk[bT_]])
                    tr(banks[bT_][:, 384:512], lt[:, 320:448], ident[:], ['lt', 'ident'], [pk[bT_]])
                    cp('dve', ltT[0:96, 0:2, :].rearrange("p a t -> p (a t)"), banks[bT_][0:96, 0:256], [pk[bT_]], ['ltT'])
                    cp('dve', ltT[:, 2:4, :].rearrange("p a t -> p (a t)"), banks[bT_][:, 256:512], [pk[bT_]], ['ltT'])
                    for half in range(2):
                        b_ = nbk()
                        mm(banks[b_][:], ltT[0:96, 0, :], wup[:, half * 512:(half + 1) * 512], True, True, ['ltT', 'wup'], [pk[b_]])
                        tt('dve', A1[:, half * 512:(half + 1) * 512], banks[b_][:], cvec["w0"][:, half * 512:(half + 1) * 512], ALU.add, [pk[b_], 'c_w0'], ['A1'])
                        b_ = nbk()
                        mm(banks[b_][:], ltT[0:96, 1, :], aup[:, half * 512:(half + 1) * 512], True, True, ['ltT', 'aup'], [pk[b_]])
                        tt('dve', A2[:, half * 512:(half + 1) * 512], banks[b_][:], cvec["a0"][:, half * 512:(half + 1) * 512], ALU.add, [pk[b_], 'c_a0'], ['A2'])
                        b_ = nbk()
                        mm(banks[b_][:], ltT[:, 2, :], gup[:, 0, half * 512:(half + 1) * 512], True, False, ['ltT', 'gup'], [pk[b_]])
                        mm(banks[b_][:], ltT[:, 3, :], gup[:, 1, half * 512:(half + 1) * 512], False, True, ['ltT', 'gup'], [pk[b_]])
                        cp('act', A3[:, half * 512:(half + 1) * 512], banks[b_][:], [pk[b_]], ['A3'])
                    act(A1[:], A1[:], AF.Sigmoid, ['A1'], ['A1'])
                    ts('pool', A1[:], A1[:], -0.6065306597126334, None, ALU.mult, None, ['A1'], ['A1'])
                    act(A2[:], A2[:], AF.Sigmoid, ['A2'], ['A2'])
                    tt('dve', A4[:], Kb[:], cvec["k_k"][:], ALU.mult, ['Kb', 'c_k_k'], ['A4'])
                    tt('pool', TMP[:], A4[:], A4[:], ALU.mult, ['A4'], ['TMP'])
                    S.op('dve', lambda: V_.tensor_reduce(out=st16[:], in_=h3(TMP[:]), axis=AX.X, op=ALU.add), ['TMP'], ['st16'])
                    act(st16[:], st16[:], AF.Sqrt, ['st16'], ['st16'])
                    ts('dve', st16[:], st16[:], 1e-12, None, ALU.max, None, ['st16'], ['st16'])
                    S.op('dve', lambda: V_.reciprocal(out=st16[:], in_=st16[:]), ['st16'], ['st16'])
                    tt('dve', h3(A4[:]), h3(A4[:]), bc16(st16[:]), ALU.mult, ['A4', 'st16'], ['A4'])
                    stt(TMP[:], A2[:], -1.0, cvec["k_a"][:], ALU.add, ALU.mult, ['A2', 'c_k_a'], ['TMP'])
                    stt(A5[:], TMP[:], 1.0, Kb[:], ALU.add, ALU.mult, ['TMP', 'Kb'], ['A5'])
                    tt('pool', A6[:], A4[:], A2[:], ALU.mult, ['A4', 'A2'], ['A6'])
                    bcw = [nbk(), nbk()]
                    bcc = [nbk(), nbk()]
                    for half in range(2):
                        mm(banks[bcw[half]][:], tri2[:], A1[:, half * 512:(half + 1) * 512], True, True, ['tri2', 'A1'], [pk[bcw[half]]])
                        mm(banks[bcc[half]][:], blk2[:], A1[:, half * 512:(half + 1) * 512], True, True, ['blk2', 'A1'], [pk[bcc[half]]])
                    for half in range(2):
                        sl = slice(half * 512, (half + 1) * 512)
                        cw = banks[bcw[half]][:]
                        cc = banks[bcc[half]][:]
                        kw_ = pk[bcw[half]]
                        kc_ = pk[bcc[half]]
                        act(TMP[:, sl], cw, AF.Exp, [kw_], ['TMP'])
                        tt('dve', tm["rh"][:, sl], Rb[:, sl], TMP[:, sl], ALU.mult, ['Rb', 'TMP'], ['t_rh'])
                        act(TMP[:, sl], cw, AF.Exp, [kw_], ['TMP'], scale=-1.0)
                        tt('dve', tm["bh"][:, sl], A6[:, sl], TMP[:, sl], ALU.mult, ['A6', 'TMP'], ['t_bh'])
                        tt('pool', tm["kh"][:, sl], A5[:, sl], TMP[:, sl], ALU.mult, ['A5', 'TMP'], ['t_kh'])
                        tt('dve', TMP[:, sl], cw, A1[:, sl], ALU.subtract, [kw_, 'A1'], ['TMP'])
                        act(TMP[:, sl], TMP[:, sl], AF.Exp, ['TMP'], ['TMP'])
                        stt(tm["ah"][:, sl], A4[:, sl], -1.0, TMP[:, sl], ALU.mult, ALU.mult, ['A4', 'TMP'], ['t_ah'])
                        cp('act', Kb[:, sl], cw, [kw_], ['Kb'])
                        tt('dve', TMP[:, sl], cc, Kb[:, sl], ALU.subtract, [kc_, 'Kb'], ['TMP'])
                        act(TMP[:, sl], TMP[:, sl], AF.Exp, ['TMP'], ['TMP'])
                        tt('dve', tm["bb"][:, sl], A6[:, sl], TMP[:, sl], ALU.mult, ['A6', 'TMP'], ['t_bb'])
                        tt('pool', tm["kb"][:, sl], A5[:, sl], TMP[:, sl], ALU.mult, ['A5', 'TMP'], ['t_kb'])
                        act(TMP[:, sl], cc, AF.Exp, [kc_], ['TMP'])
                        tt('dve', h3(tm["Fm"][:, sl]), h3(TMP[:, sl]), idp[:].unsqueeze(1).to_broadcast([128, 8, 64]), ALU.mult, ['TMP', 'idp'], ['t_Fm'])
                    cp('pool', tm["vb"][:], Vb[:], ['Vb'], ['t_vb'])
                    for nm in ("ah", "vb", "bb", "kb", "Fm"):
                        dma('sp', sh[nm][:], tm[nm][64:128, :], ['t_' + nm], ['s_' + nm])
                    for xi, nm in enumerate(("rh", "ah", "bh", "kh")):
                        if nm == "rh" and not own:
                            continue
                        for hg in range(2):
                            b_ = nbk()
                            pbv = banks[b_][:].bitcast(BF16)
                            for hh in range(8):
                                h = hg * 8 + hh
                                tr(pbv[0:64, hh * 128:(hh + 1) * 128], tm[nm][:, h * 64:(h + 1) * 64], identb[:], ['t_' + nm, 'identb'], [pk[b_]])
                            src = pbv[0:64, :].rearrange("p (h t) -> p h t", t=128)
                            if (xi + hg) % 2:
                                act(FM[:, hg * 8:(hg + 1) * 8, xi, :], src, AF.Copy, [pk[b_]], ['FM'])
                            else:
                                cp('dve', FM[:, hg * 8:(hg + 1) * 8, xi, :], src, [pk[b_]], ['FM'])
                    for c in range(2):
                        cs = slice(c * 64, (c + 1) * 64)
                        gchunk = tile * 2 + c
                        if gchunk == 32:
                            ts('dve', Zb[:], Zb[:], flagcol[0:64, 0:1], None, ALU.mult, None, ['Zb', 'flagcol'], ['Zb'])

                        def chunk_steps(hg, c=c, cs=cs):
                            B = CB[hg]
                            hs = [hg * 8 + hh for hh in range(8)]
                            evi = [hg]

                            def tmop(nm, h):
                                if c == 0:
                                    return tm[nm][0:64, h * 64:(h + 1) * 64], 't_' + nm
                                return sh[nm][:, h * 64:(h + 1) * 64], 's_' + nm

                            def evac(dst, in_, key_in, mask=None):
                                out, key_out = dst
                                evi[0] += 1
                                o2 = out.rearrange("p h c -> p (h c)")
                                if mask is not None:
                                    tt('dve', o2, in_, mask, ALU.mult, [key_in, 'cmb'], [key_out])
                                elif evi[0] % 2:
                                    act(o2, in_, AF.Copy, [key_in], [key_out])
                                else:
                                    cp('dve', o2, in_, [key_in], [key_out])

                            def fmT(xi, h):
                                return FM[:, h, xi, cs]

                            def R_(ap):
                                return ap.bitcast(F32R)

                            def evacr(dst, in_, key_in):
                                out, key_out = dst
                                evi[0] += 1
                                o2 = R_(out).rearrange("p h c -> p (h c)")
                                if evi[0] % 2:
                                    act(o2, in_, AF.Copy, [key_in], [key_out])
                                else:
                                    tt('dve', o2, in_, onesw[:], ALU.mult, [key_in, 'onesw'], [key_out])

                            def group(lh_fn, rh_fn, rkeys):
                                b_ = nbk()
                                for hh, h in enumerate(hs):
                                    mm(banks[b_][0:64, hh * 64:(hh + 1) * 64], lh_fn(hh, h), rh_fn(hh, h), True, True, rkeys, [pk[b_]])
                                return b_

                            RH, AH, BH, KH = 0, 1, 2, 3
                            Lb_, Lpb_, TTb_ = B['Lb'], B['Lpb'], B['TTb']
                            b_ = group(lambda hh, h: fmT(BH, h), lambda hh, h: fmT(AH, h), ['FM'])
                            tt('dve', R_(Lpb_[0][0]).rearrange("p h c -> p (h c)"), banks[b_][0:64, :], cmf[:, 0, :], ALU.mult, [pk[b_], 'cmf'], [Lpb_[0][1]])
                            yield
                            b_ = group(lambda hh, h: fmT(AH, h), lambda hh, h: fmT(BH, h), ['FM'])
                            tt('dve', R_(Lb_[0][0]).rearrange("p h c -> p (h c)"), banks[b_][0:64, :], cmf[:, 2, :], ALU.mult, [pk[b_], 'cmf'], [Lb_[0][1]])
                            tt('dve', R_(TTb_[0][0]).rearrange("p h c -> p (h c)"), Lpb_[0][0].rearrange("p h c -> p (h c)"),
                               irepf[:].rearrange("p h c -> p (h c)"), ALU.add, [Lpb_[0][1], 'irepf'], [TTb_[0][1]])
                            yield
                            if own:
                                b_ = group(lambda hh, h: fmT(BH, h), lambda hh, h: fmT(RH, h), ['FM'])
                                evac(B['MrbT'], banks[b_][0:64, :], pk[b_], mask=MU_I)
                                yield
                            b_ = group(lambda hh, h: fmT(KH, h), lambda hh, h: fmT(AH, h), ['FM'])
                            evac(B['MakT'], banks[b_][0:64, :], pk[b_], mask=MU_S)
                            yield
                            if own:
                                b_ = group(lambda hh, h: fmT(KH, h), lambda hh, h: fmT(RH, h), ['FM'])
                                evac(B['MrkT'], banks[b_][0:64, :], pk[b_], mask=MU_I)
                                yield
                            for it in range(5):
                                ci, ni = it % 2, (it + 1) % 2
                                b1 = group(lambda hh, h: R_(Lpb_[ci][0])[:, hh, :], lambda hh, h: R_(Lb_[ci][0])[:, hh, :], [Lpb_[ci][1], Lb_[ci][1]])
                                evacr(Lb_[ni], banks[b1][0:64, :], pk[b1])
                                yield
                                if it < 4:
                                    b2 = group(lambda hh, h: R_(Lb_[ci][0])[:, hh, :], lambda hh, h: R_(Lpb_[ci][0])[:, hh, :], [Lpb_[ci][1], Lb_[ci][1]])
                                    evacr(Lpb_[ni], banks[b2][0:64, :], pk[b2])
                                    yield
                                b3 = nbk()
                                for hh, h in enumerate(hs):
                                    mm(banks[b3][0:64, hh * 64:(hh + 1) * 64], R_(Lb_[ni][0])[:, hh, :], R_(TTb_[ci][0])[:, hh, :], True, True, [Lb_[ni][1], TTb_[ci][1]], [pk[b3]])
                                tt('dve', R_(TTb_[ni][0]).rearrange("p h c -> p (h c)"), banks[b3][0:64, :], TTb_[ci][0].rearrange("p h c -> p (h c)"),
                                   ALU.add, [pk[b3], TTb_[ci][1]], [TTb_[ni][1]])
                                yield
                            TTf, kTT = B['TTfb']
                            cp('pool', TTf.rearrange("p h c -> p (h c)"), TTb_[1][0].rearrange("p h c -> p (h c)"), [TTb_[1][1]], [kTT])
                            MakT_, MrbT_, MrkT_ = B['MakT'], B['MrbT'], B['MrkT']
                            Wb_, Atb_, Vtb_, RtT_, Pmb_ = B['Wb'], B['Atb'], B['Vtb'], B['RtT'], B['Pmb']
                            b_ = group(lambda hh, h: MakT_[0][:, hh, :], lambda hh, h: tmop("vb", h)[0], [MakT_[1], tmop("vb", 0)[1]])
                            evac(Wb_, banks[b_][0:64, :], pk[b_])
                            yield
                            b_ = group(lambda hh, h: TTf[:, hh, :], lambda hh, h: tmop("ah", h)[0], [kTT, tmop("ah", 0)[1]])
                            evac(Atb_, banks[b_][0:64, :], pk[b_])
                            yield
                            b_ = group(lambda hh, h: TTf[:, hh, :], lambda hh, h: Wb_[0][:, hh, :], [kTT, Wb_[1]])
                            evac(Vtb_, banks[b_][0:64, :], pk[b_])
                            yield
                            if own:
                                b_ = group(lambda hh, h: Atb_[0][:, hh, :], lambda hh, h: MrbT_[0][:, hh, :], [Atb_[1], MrbT_[1]])
                                tt('dve', RtT_[0], banks[b_][0:64, :].rearrange("p (h c) -> p h c", c=64), FM[:, hg * 8:(hg + 1) * 8, RH, cs],
                                   ALU.add, [pk[b_], 'FM'], [RtT_[1]])
                                yield
                                b_ = nbk()
                                for hh, h in enumerate(hs):
                                    o = banks[b_][0:64, hh * 64:(hh + 1) * 64]
                                    mm(o, MrbT_[0][:, hh, :], Vtb_[0][:, hh, :], True, False, [MrbT_[1], Vtb_[1]], [pk[b_]])
                                    mm(o, MrkT_[0][:, hh, :], tmop("vb", h)[0], False, False, [MrkT_[1], tmop("vb", h)[1]], [pk[b_]])
                                    mm(o, RtT_[0][:, hh, :], Zb[:, h, :], False, True, [RtT_[1], 'Zb'], [pk[b_]])
                                if c == 0:
                                    cp('dve', Yt[0:64, hg * 512:(hg + 1) * 512], banks[b_][0:64, :], [pk[b_]], ['Yt'])
                                else:
                                    act(Ych[:, hg * 512:(hg + 1) * 512], banks[b_][0:64, :], AF.Copy, [pk[b_]], ['TMP'])
                                yield
                            b_ = nbk()
                            for hh, h in enumerate(hs):
                                o = banks[b_][0:64, hh * 64:(hh + 1) * 64]
                                mm(o, Atb_[0][:, hh, :], tmop("bb", h)[0], True, True, [Atb_[1], tmop("bb", h)[1]], [pk[b_]])
                            fmsrc = (tm["Fm"][0:64, hg * 512:(hg + 1) * 512], 't_Fm') if c == 0 else (sh["Fm"][:, hg * 512:(hg + 1) * 512], 's_Fm')
                            tt('dve', Pmb_[0].rearrange("p h c -> p (h c)"), banks[b_][0:64, :], fmsrc[0], ALU.add, [pk[b_], fmsrc[1]], [Pmb_[1]])
                            yield
                            b_ = nbk()
                            for hh, h in enumerate(hs):
                                o = banks[b_][0:64, hh * 64:(hh + 1) * 64]
                                mm(o, Pmb_[0][:, hh, :], Zb[:, h, :], True, False, [Pmb_[1], 'Zb'], [pk[b_]])
                                mm(o, tmop("bb", h)[0], Vtb_[0][:, hh, :], False, False, [tmop("bb", h)[1], Vtb_[1]], [pk[b_]])
                                mm(o, tmop("kb", h)[0], tmop("vb", h)[0], False, True, [tmop("kb", h)[1], tmop("vb", h)[1]], [pk[b_]])
                            cp('dve', Zb[:, hg * 8:(hg + 1) * 8, :].rearrange("p h c -> p (h c)"), banks[b_][0:64, :], [pk[b_]], ['Zb'])
                            yield

                        gens = [chunk_steps(0), chunk_steps(1)]
                        live = [True, True]
                        while any(live):
                            for gi_ in range(2):
                                if live[gi_]:
                                    try:
                                        next(gens[gi_])
                                    except StopIteration:
                                        live[gi_] = False
                        if own and c == 1:
                            dma('sp', Yt[64:128, :], Ych, ['TMP'], ['Yt'])
                    if not own:
                        continue
                    S.op('dve', lambda: V_.tensor_reduce(out=st16[:], in_=h3(Yt[:]), axis=AX.X, op=ALU.add), ['Yt'], ['st16'])
                    ts('dve', st16[:], st16[:], 1.0 / 64, None, ALU.mult, None, ['st16'], ['st16'])
                    tt('dve', h3(Yt[:]), h3(Yt[:]), bc16(st16[:]), ALU.subtract, ['Yt', 'st16'], ['Yt'])
                    tt('pool', TMP[:], Yt[:], Yt[:], ALU.mult, ['Yt'], ['TMP'])
                    S.op('dve', lambda: V_.tensor_reduce(out=st16b[:], in_=h3(TMP[:]), axis=AX.X, op=ALU.add), ['TMP'], ['st16b'])
                    ts('dve', st16b[:], st16b[:], 1.0 / 64, GN_EPS, ALU.mult, ALU.add, ['st16b'], ['st16b'])
                    act(st16b[:], st16b[:], AF.Sqrt, ['st16b'], ['st16b'])
                    S.op('dve', lambda: V_.reciprocal(out=st16b[:], in_=st16b[:]), ['st16b'], ['st16b'])
                    tt('dve', h3(Yt[:]), h3(Yt[:]), bc16(st16b[:]), ALU.mult, ['Yt', 'st16b'], ['Yt'])
                    tt('pool', Yt[:], Yt[:], cvec["ln_w"][:], ALU.mult, ['Yt', 'c_ln_w'], ['Yt'])
                    tt('pool', Yt[:], Yt[:], cvec["ln_b"][:], ALU.add, ['Yt', 'c_ln_b'], ['Yt'])
                    tt('dve', TMP[:], Rb[:], A5[:], ALU.mult, ['Rb', 'A5'], ['TMP'])
                    tt('pool', TMP[:], TMP[:], cvec["r_k"][:], ALU.mult, ['TMP', 'c_r_k'], ['TMP'])
                    S.op('dve', lambda: V_.tensor_reduce(out=st16[:], in_=h3(TMP[:]), axis=AX.X, op=ALU.add), ['TMP'], ['st16'])
                    tt('dve', h3(TMP[:]), h3(Vb[:]), bc16(st16[:]), ALU.mult, ['Vb', 'st16'], ['TMP'])
                    tt('pool', Yt[:], Yt[:], TMP[:], ALU.add, ['Yt', 'TMP'], ['Yt'])
                    tt('dve', yb[:], Yt[:], A3[:], ALU.mult, ['Yt', 'A3'], ['yb'])
                    b_ = nbk()
                    pbv = banks[b_][:].bitcast(BF16)
                    for kk in range(8):
                        tr(pbv[:, kk * 128:(kk + 1) * 128], yb[:, kk * 128:(kk + 1) * 128], identb[:], ['yb', 'identb'], [pk[b_]])
                    cp('dve', mst[:].rearrange("p k t -> p (k t)"), pbv, [pk[b_]], ['mst'])
                    t16 = tile - 16
                    dma('sp', mixT_d[8:16, :, t16 * 128:(t16 + 1) * 128].rearrange("k p t -> p k t"), mst[:], ['mst'], ['mixT_d'])
            S.barrier()

        if "dbg_mixT_d" in dbg:
            dma('sp', dbg["dbg_mixT_d"], mixT_d, ['mixT_d'], ['dbg_mixT_d'])
        if "dbg_mixF" in dbg:
            dma('sp', dbg["dbg_mixF"], mixT_d[0:8], ['mixT_d'], ['dbg_mixF'])
        if "dbg_mixR" in dbg:
            dma('sp', dbg["dbg_mixR"], mixT_d[8:16], ['mixT_d'], ['dbg_mixR'])

        if "E" in stages:
            with ExitStack() as ph:
                wo = sb(ph, "wo", [128, KD, D], BF16)
                g1bc = sb(ph, "g1bc", [128, D], F32)
                gtmp = sb(ph, "gtmp", [128, 128], F32)
                mx = [sb(ph, f"mx{i}", [128, KD, 128], BF16) for i in range(2)]
                xo = [sb(ph, f"xo{i}", [128, D], F32) for i in range(2)]
                x1 = [sb(ph, f"x1{i}", [128, D], F32) for i in range(2)]
                junk2 = sb(ph, "junk2", [128, D], BF16)
                xn2 = sb(ph, "xn2", [128, D], BF16)
                ss2 = sb(ph, "ss2", [128, 16], F32)
                rstd2 = sb(ph, "rstd2", [128, 16], F32)
                scl2 = sb(ph, "scl2", [128, KD], F32)
                nw2 = sb(ph, "nw2", [128, KD], F32)
                h2s = [sb(ph, f"h2s{i}", [128, KD, 128], BF16) for i in range(2)]
                wov = I["w_out"].rearrange("(k p) c -> p k c", p=128)
                for q4 in range(4):
                    dma('pool', wo[:, q4 * 4:(q4 + 1) * 4, :], wov[:, q4 * 4:(q4 + 1) * 4, :], [], ['wo'])
                dma('sp', nw2[:], I["nw2T"], [], ['nw2'])
                stt(scl2[:], sc2, 1.0, nw2[:], ALU.add, ALU.mult, ['mod', 'nw2'], ['scl2'])
                for k in range(KD):
                    ts('dve', gtmp[:], ones[:], g1c[:, k:k + 1], None, ALU.mult, None, ['ones', 'mod'], ['gtmp'])
                    mm(banks[0][:, 0:128], gtmp[:], ident[:], True, True, ['gtmp', 'ident'], [pk[0]])
                    cp('dve', g1bc[:, k * 128:(k + 1) * 128], banks[0][:, 0:128], [pk[0]], ['g1bc'])
                mview = mixT_d.rearrange("k p t -> p k t")
                for t16 in range(16):
                    m_ = mx[t16 % 2]
                    mk = f"mx{t16 % 2}"
                    xo_ = xo[t16 % 2]
                    xk = f"xo{t16 % 2}"
                    x1_ = x1[t16 % 2]
                    x1k = f"x1{t16 % 2}"
                    dma('sp', m_[:], mview[:, :, t16 * 128:(t16 + 1) * 128], ['mixT_d'], [mk])
                    dma('sp', xo_[:], I["xc"][OWN + t16 * 128:OWN + (t16 + 1) * 128, :], [], [xk])
                    for nb in range(4):
                        bk = 1 + nb
                        for k in range(KD):
                            mm(banks[bk][:], m_[:, k, :], wo[:, k, nb * 512:(nb + 1) * 512], k == 0, k == KD - 1, [mk, 'wo'], [pk[bk]])
                        tt('dve', x1_[:, nb * 512:(nb + 1) * 512], banks[bk][:], g1bc[:, nb * 512:(nb + 1) * 512], ALU.mult, [pk[bk], 'g1bc'], [x1k])
                        tt('pool', x1_[:, nb * 512:(nb + 1) * 512], x1_[:, nb * 512:(nb + 1) * 512], xo_[:, nb * 512:(nb + 1) * 512], ALU.add, [x1k, xk], [x1k])
                    dma('sp', x1_d[t16 * 128:(t16 + 1) * 128, :], x1_[:], [x1k], ['x1_d'])
                    act(junk2[:], x1_[:], AF.Square, [x1k], ['junk2'], accum_out=ss2[:, t16:t16 + 1])
                    ts('dve', rstd2[:, t16:t16 + 1], ss2[:, t16:t16 + 1], 1.0 / D, NORM_EPS, ALU.mult, ALU.add, ['junk2'], ['rstd2'])
                    act(rstd2[:, t16:t16 + 1], rstd2[:, t16:t16 + 1], AF.Sqrt, ['rstd2'], ['rstd2'])
                    S.op('dve', lambda: V_.reciprocal(out=rstd2[:, t16:t16 + 1], in_=rstd2[:, t16:t16 + 1]), ['rstd2'], ['rstd2'])
                    act(xn2[:], x1_[:], AF.Copy, [x1k, 'rstd2'], ['xn2'], scale=rstd2[:, t16:t16 + 1])
                    hs = h2s[t16 % 2]
                    hk = f"h2s{t16 % 2}"
                    for half in range(2):
                        pbv = banks[5 + half][:].bitcast(BF16)
                        for kk in range(8):
                            k = half * 8 + kk
                            tr(pbv[:, kk * 128:(kk + 1) * 128], xn2[:, k * 128:(k + 1) * 128], identb[:], ['xn2', 'identb'], [pk[5 + half]])
                        for kk in range(8):
                            k = half * 8 + kk
                            if kk % 2 == 0:
                                ts('dve', hs[:, k, :], pbv[:, kk * 128:(kk + 1) * 128], scl2[:, k:k + 1], sh2[:, k:k + 1], ALU.mult, ALU.add,
                                   [pk[5 + half], 'scl2', 'mod'], [hk])
                            else:
                                act(hs[:, k, :], pbv[:, kk * 128:(kk + 1) * 128], AF.Identity, [pk[5 + half], 'scl2', 'mod'], [hk],
                                    scale=scl2[:, k:k + 1], bias=sh2[:, k:k + 1])
                    dma('sp', h2T_d[:, :, t16 * 128:(t16 + 1) * 128], hs[:], [hk], ['h2T_d'])
            S.barrier()
        if "dbg_x1_d" in dbg:
            dma('sp', dbg["dbg_x1_d"], x1_d, ['x1_d'], ['dbg_x1_d'])
        if "dbg_h2T_d" in dbg:
            dma('sp', dbg["dbg_h2T_d"], h2T_d, ['h2T_d'], ['dbg_h2T_d'])

        if "F" in stages:
            with ExitStack() as ph:
                wq = sb(ph, "wq", [128, KD, D], BF16)
                h2b0 = [sb(ph, f"h2b0{i}", [128, KD, 128], BF16) for i in range(2)]
                qT0 = [sb(ph, f"qT0{i}", [128, 16, 128], F32) for i in range(2)]
                wqv = I["w_query"].rearrange("(k p) c -> p k c", p=128)
                for q4 in range(4):
                    dma('pool', wq[:, q4 * 4:(q4 + 1) * 4, :], wqv[:, q4 * 4:(q4 + 1) * 4, :], [], ['wq'])
                for tb in range(16):
                    hb = h2b0[tb % 2]
                    hbk = f"h2b0{tb % 2}"
                    qo = qT0[tb % 2]
                    qk = f"qT0{tb % 2}"
                    dma('sp', hb[:], h2T_d[:, :, tb * 128:(tb + 1) * 128], ['h2T_d'], [hbk])
                    for hp in range(16):
                        bk = (hp // 4) + 4 * (tb % 2)
                        for k in range(KD):
                            mm(banks[bk][:, (hp % 4) * 128:(hp % 4 + 1) * 128], wq[:, k, hp * 128:(hp + 1) * 128], hb[:, k, :],
                               k == 0, k == KD - 1, ['wq', hbk], [pk[bk]])
                    for b4 in range(4):
                        bk = b4 + 4 * (tb % 2)
                        o = qo[:, b4 * 4:(b4 + 1) * 4, :].rearrange("p a t -> p (a t)")
                        if b4 % 2:
                            act(o, banks[bk][:], AF.Copy, [pk[bk]], [qk])
                        else:
                            cp('dve', o, banks[bk][:], [pk[bk]], [qk])
                    dma('sp', qp_d[:, :, tb * 128:(tb + 1) * 128], qo[:], [qk], ['qp_d'])
            S.barrier()

        if "F" in stages:
            with ExitStack() as ph:
                keysT = sb(ph, "keysT", [128, 16, 128], F32)
                iota = sb(ph, "iota", [128, 128], F32)
                bmask = sb(ph, "bmask", [128, 8], F32)
                onec = sb(ph, "onecF", [128, 1], F32)
                g2bc = sb(ph, "g2bc", [128, D], F32)
                gtmp = sb(ph, "gtmp2", [128, 128], F32)
                h2blk = sb(ph, "h2blk", [128, KD, 256], BF16)
                bufA = sb(ph, "bufA", [128, 2048], F32)
                bufB = sb(ph, "bufB", [128, 2048], F32)
                bufC = sb(ph, "bufC", [128, 2048], F32)
                bufD = sb(ph, "bufD", [128, 2048], F32)
                bufE = sb(ph, "bufE", [128, 2048], F32)
                scs = bufA[:].rearrange("p (a t) -> p a t", t=128)
                scr = bufC[:].rearrange("p (a t) -> p a t", t=128)
                cand = bufB[:].rearrange("p (h c) -> p h c", c=256)
                scr2 = bufC[:].rearrange("p (h c) -> p h c", c=256)
                ee = bufD[:].rearrange("p (h c) -> p h c", c=256)
                qTs = bufE[:].rearrange("p (a t) -> p a t", t=128)
                gT = bufE[:].rearrange("p (a t) -> p a t", t=128)
                top = sb(ph, "top", [128, 16, 16], F32)
                idx = sb(ph, "idx", [128, 16, 16], U32)
                idxf0 = sb(ph, "idxf0", [128, 8, 16], F32)
                idxf1 = sb(ph, "idxf1", [128, 8, 16], F32)
                c8a = sb(ph, "c8a", [128, 8, 8], F32)
                c8b = sb(ph, "c8b", [128, 8, 8], F32)
                zz = sb(ph, "zz", [128, 8], F32)
                gateP = sb(ph, "gateP", [128, 16, 8, 16], F32)
                JTs = sb(ph, "JTs", [128, 128], F32)
                dJ1 = sb(ph, "dJ0", [128, 16, 128], BF16)
                dJ = [dJ1, dJ1]
                OHI = [sb(ph, f"OHI{i}", [128, 16, 128], BF16) for i in range(2)]
                OHJ = [sb(ph, f"OHJ{i}", [128, 16, 128], BF16) for i in range(2)]
                GTb = [sb(ph, f"GTb{i}", [128, 16, 8, 16], BF16) for i in range(2)]
                Bs = [sb(ph, f"Bs{i}", [128, 16, 128], BF16) for i in range(2)]
                Gsb = sb(ph, "Gsb", [128, 128, 256], BF16)
                NB_ = 4
                uTt = [sb(ph, f"uTt{i}", [128, KD, 128], BF16) for i in range(NB_)]
                vh = [sb(ph, f"vh{i}", [128, 1024], BF16) for i in range(NB_)]
                gel = [sb(ph, f"gel{i}", [128, 256], F32) for i in range(2)]
                dma('sp', keysT[:], I["keysT"], [], ['keysT'])
                dma('sp', iota[:], I["iota"], [], ['iota'])
                dma('sp', bmask[:], I["blockmask"], [], ['bmask'])
                S.op('dve', lambda: V_.memset(onec[:], 1.0), [], ['onecF'])
                for k in range(KD):
                    ts('dve', gtmp[:], ones[:], g2c[:, k:k + 1], None, ALU.mult, None, ['ones', 'mod'], ['gtmp2'])
                    mm(banks[0][:, 0:128], gtmp[:], ident[:], True, True, ['gtmp2', 'ident'], [pk[0]])
                    cp('dve', g2bc[:, k * 128:(k + 1) * 128], banks[0][:, 0:128], [pk[0]], ['g2bc'])
                top4 = top[:].rearrange("p (h two) a -> p h two a", two=2)
                idx4 = idx[:].rearrange("p (h two) a -> p h two a", two=2)
                cand4 = cand.rearrange("p h (a b) -> p h a b", b=16)
                ee4 = ee.rearrange("p h (a b) -> p h a b", b=16)
                nld = 0
                nsub = 0
                for tb in range(8):
                    dma('sp', h2blk[:], h2T_d[:, :, tb * 256:(tb + 1) * 256], ['h2T_d'], ['h2blk'])
                    for half in range(2):
                        tk0 = tb * 256 + half * 128
                        dma('sp', qTs, qp_d[:, :, tk0:tk0 + 128], ['qp_d'], ['bufE'])
                        for hp in range(16):
                            bk = hp // 4
                            mm(banks[bk][:, (hp % 4) * 128:(hp % 4 + 1) * 128], qTs[:, hp, :], keysT[:, hp, :], True, True, ['bufE', 'keysT'], [pk[bk]])
                        for bk in range(4):
                            o = scs[:, bk * 4:(bk + 1) * 4, :].rearrange("p a t -> p (a t)")
                            if bk % 2:
                                act(o, banks[bk][:], AF.Copy, [pk[bk]], ['bufA'])
                            else:
                                cp('dve', o, banks[bk][:], [pk[bk]], ['bufA'])
                        for hp in range(16):
                            S.op('dve', lambda: V_.max(out=top[:, hp, 0:8], in_=scs[:, hp, :]), ['bufA'], ['top'])
                            S.op('dve', lambda: V_.match_replace(out=scr[:, hp, :], in_to_replace=top[:, hp, 0:8], in_values=scs[:, hp, :], imm_value=-1e30),
                                 ['bufA', 'top'], ['bufC'])
                            S.op('dve', lambda: V_.max(out=top[:, hp, 8:16], in_=scr[:, hp, :]), ['bufC'], ['top'])
                            S.op('dve', lambda: V_.max_index(out=idx[:, hp, 0:8], in_max=top[:, hp, 0:8], in_values=scs[:, hp, :]), ['bufA', 'top'], ['idx'])
                            S.op('dve', lambda: V_.max_index(out=idx[:, hp, 8:16], in_max=top[:, hp, 8:16], in_values=scs[:, hp, :]), ['bufA', 'top'], ['idx'])
                        cp('dve', idxf0[:], idx4[:, :, 0, :], ['idx'], ['idxf0'])
                        cp('dve', idxf1[:], idx4[:, :, 1, :], ['idx'], ['idxf1'])
                        tt('dve', cand4, top4[:, :, 0, :].unsqueeze(3).to_broadcast([128, 8, 16, 16]),
                           top4[:, :, 1, :].unsqueeze(2).to_broadcast([128, 8, 16, 16]), ALU.add, ['top'], ['bufB'])
                        for h in range(8):
                            S.op('dve', lambda: V_.max(out=c8a[:, h, :], in_=cand[:, h, :]), ['bufB'], ['c8a'])
                            S.op('dve', lambda: V_.match_replace(out=scr2[:, h, :], in_to_replace=c8a[:, h, :], in_values=cand[:, h, :], imm_value=-1e30),
                                 ['bufB', 'c8a'], ['bufC'])
                            S.op('dve', lambda: V_.max(out=c8b[:, h, :], in_=scr2[:, h, :]), ['bufC'], ['c8b'])
                        tt('dve', ee, cand, c8a[:, :, 0:1].to_broadcast([128, 8, 256]), ALU.subtract, ['bufB', 'c8a'], ['bufD'])
                        act(ee, ee, AF.Exp, ['bufD'], ['bufD'])
                        tt('dve', scr2, cand, c8b[:, :, 7:8].to_broadcast([128, 8, 256]), ALU.is_ge, ['bufB', 'c8b'], ['bufC'])
                        tt('dve', ee, ee, scr2, ALU.mult, ['bufD', 'bufC'], ['bufD'])
                        S.op('dve', lambda: V_.tensor_reduce(out=zz[:], in_=ee, axis=AX.X, op=ALU.add), ['bufD'], ['zz'])
                        S.op('dve', lambda: V_.reciprocal(out=zz[:], in_=zz[:]), ['zz'], ['zz'])
                        tt('dve', gateP[:].rearrange("p a h b -> p h a b"), ee4, zz[:].unsqueeze(2).unsqueeze(3).to_broadcast([128, 8, 16, 16]),
                           ALU.mult, ['bufD', 'zz'], ['gateP'])
                        tr(banks[0][:, 0:128], idxf0[:].rearrange("p h a -> p (h a)"), ident[:], ['idxf0', 'ident'], [pk[0]])
                        tr(banks[0][:, 128:256], idxf1[:].rearrange("p h a -> p (h a)"), ident[:], ['idxf1', 'ident'], [pk[0]])
                        act(JTs[:], banks[0][:, 128:256], AF.Copy, [pk[0]], ['JTs'])
                        for a in range(16):
                            bk = 4 + a // 4
                            tr(banks[bk][:, (a % 4) * 128:(a % 4 + 1) * 128], gateP[:, a, :, :].rearrange("p h b -> p (h b)"), ident[:],
                               ['gateP', 'ident'], [pk[bk]])
                        for b4 in range(4):
                            o = gT[:, b4 * 4:(b4 + 1) * 4, :].rearrange("p a t -> p (a t)")
                            if b4 % 2:
                                act(o, banks[4 + b4][:], AF.Copy, [pk[4 + b4]], ['bufE'])
                            else:
                                cp('dve', o, banks[4 + b4][:], [pk[4 + b4]], ['bufE'])
                        for sub in range(8):
                            t0 = sub * 16
                            si = nsub % 2
                            nsub += 1
                            tt('pool', GTb[si][:], gT[:, :, t0:t0 + 16].rearrange("p a t -> p t a").unsqueeze(2).to_broadcast([128, 16, 8, 16]),
                               bmask[:].unsqueeze(1).unsqueeze(3).to_broadcast([128, 16, 8, 16]), ALU.mult, ['bufE', 'bmask'], [f'GTb{si}'])
                            tt('dve', OHI[si][:], iota[:].unsqueeze(1).to_broadcast([128, 16, 128]),
                               banks[0][:, t0:t0 + 16].unsqueeze(2).to_broadcast([128, 16, 128]), ALU.is_equal, ['iota', pk[0]], [f'OHI{si}'])
                            tt('pool', dJ[si][:], iota[:].unsqueeze(1).to_broadcast([128, 16, 128]),
                               JTs[:, t0:t0 + 16].unsqueeze(2).to_broadcast([128, 16, 128]), ALU.subtract, ['iota', 'JTs'], ['dJ0'])
                            act(dJ[si][:], dJ[si][:], AF.Square, ['dJ0'], ['dJ0'])
                            act(OHJ[si][:], dJ[si][:], AF.Relu, ['dJ0', 'onecF'], [f'OHJ{si}'], bias=onec[:], scale=-1.0)
                            for t4 in range(4):
                                bk = 2 + t4 % 2
                                for tl in range(4):
                                    t = t4 * 4 + tl
                                    mm(banks[bk][:, tl * 128:(tl + 1) * 128], GTb[si][:, t, :, :].rearrange("p h a -> p (h a)"), OHJ[si][:, t, :], True, True,
                                       [f'GTb{si}', f'OHJ{si}'], [pk[bk]])
                                o = Bs[si][:, t4 * 4:(t4 + 1) * 4, :].rearrange("p t j -> p (t j)")
                                act(o, banks[bk][:], AF.Copy, [pk[bk]], [f'Bs{si}'])
                            for t4 in range(4):
                                bk = 4 + t4
                                for tl in range(4):
                                    t = t4 * 4 + tl
                                    mm(banks[bk][:, tl * 128:(tl + 1) * 128], Bs[si][:, t, :], OHI[si][:, t, :], True, True, [f'Bs{si}', f'OHI{si}'], [pk[bk]])
                                c0 = half * 128 + t0 + t4 * 4
                                o = Gsb[:, :, c0:c0 + 4]
                                src = banks[bk][:].rearrange("p (t i) -> p i t", i=128)
                                if t4 % 4 != 3:
                                    act(o, src, AF.Copy, [pk[bk]], ['Gsb'])
                                else:
                                    cp('dve', o, src, [pk[bk]], ['Gsb'])
                    for dpass in range(2):
                        dc = slice(dpass * 1024, (dpass + 1) * 1024)
                        for i0 in range(128):
                            bi = nld % NB_
                            nld += 1
                            vv = vh[bi]
                            vk = f"vh{bi}"
                            dma('sp', vv[:], vb_d[i0 * 128:(i0 + 1) * 128, dc], ['vb_d'], [vk])
                            if dpass == 0:
                                ut = uTt[bi]
                                utk = f"uTt{bi}"
                                dma('sp', ut[:].rearrange("p k j -> p (k j)"), uTb_d[i0], ['uTb_d'], [utk])
                                bk = 2 + i0 % 2
                                for k in range(KD):
                                    mm(banks[bk][:, 0:256], ut[:, k, :], h2blk[:, k, :], k == 0, k == KD - 1, [utk, 'h2blk'], [pk[bk]])
                                ge = gel[i0 % 2]
                                gek = f"gel{i0 % 2}"
                                act(ge[:], banks[bk][:, 0:256], AF.Gelu, [pk[bk]], [gek])
                                tt('dve', Gsb[:, i0, :], ge[:], Gsb[:, i0, :], ALU.mult, [gek, 'Gsb'], ['Gsb'])
                            for t2 in range(2):
                                for db in range(2):
                                    ob_ = 4 + t2 * 2 + db
                                    mm(banks[ob_][:], Gsb[:, i0, t2 * 128:(t2 + 1) * 128], vv[:, db * 512:(db + 1) * 512], i0 == 0, i0 == 127,
                                       ['Gsb', vk], [pk[ob_]])
                        for t2 in range(2):
                            r0 = tb * 256 + t2 * 128
                            yo_ = bufA[:, t2 * 1024:(t2 + 1) * 1024]
                            x1_ = bufB[:, t2 * 1024:(t2 + 1) * 1024]
                            dma('sp', x1_, x1_d[r0:r0 + 128, dc], ['x1_d'], ['bufB'])
                            for db in range(2):
                                ob_ = 4 + t2 * 2 + db
                                tt('dve', yo_[:, db * 512:(db + 1) * 512], banks[ob_][:], g2bc[:, dpass * 1024 + db * 512:dpass * 1024 + (db + 1) * 512],
                                   ALU.mult, [pk[ob_], 'g2bc'], ['bufA'])
                            tt('pool', yo_, yo_, x1_, ALU.add, ['bufA', 'bufB'], ['bufA'])
                            dma('sp', y[r0:r0 + 128, dc], yo_, ['bufA'], ['y'])
            S.barrier()

        S.barrier()
        if "Y0" in stages:
            with ExitStack() as ph:
                z = sb(ph, "zout", [128, D], F32)
                S.op('dve', lambda: V_.memset(z[:], 0.0), [], ['zout'])
                for i in range(16):
                    dma('sp', y[i * 128:(i + 1) * 128, :], z[:], ['zout'], ['y'])
        S.finish()
    return nc


def host_inputs(inputs):
    g = {k: np.asarray(v) for k, v in inputs.items()}
    f32 = np.float32
    x = g["x"]
    L = 0
    common = {
        "w_ada": np.ascontiguousarray(g["w_ada"][L]),
        "b_adaT": np.ascontiguousarray(g["b_ada"][L].reshape(96, 128).T),
        "nw1T": np.ascontiguousarray(g["norm_mix_w"][L].reshape(KD, 128).T),
        "w_in": np.ascontiguousarray(g["w_in"][L]),
        "qnw": np.ascontiguousarray(g["fox_q_norm_w"][L].reshape(128, 1)),
        "knw": np.ascontiguousarray(g["fox_k_norm_w"][L].reshape(128, 1)),
        "fbias": np.ascontiguousarray(g["fox_f_bias"][L]),
        "mu": np.ascontiguousarray(g["rwkv_mu"][L]),
        "w0": np.ascontiguousarray(g["rwkv_w0"][L]),
        "w_up": np.ascontiguousarray(g["rwkv_w_up"][L]),
        "a0": np.ascontiguousarray(g["rwkv_a0"][L]),
        "a_up": np.ascontiguousarray(g["rwkv_a_up"][L]),
        "g_up": np.ascontiguousarray(g["rwkv_g_up"][L]),
        "k_k": np.ascontiguousarray(g["rwkv_k_k"][L]),
        "k_a": np.ascontiguousarray(g["rwkv_k_a"][L]),
        "r_k": np.ascontiguousarray(g["rwkv_r_k"][L].reshape(1024)),
        "ln_w": np.ascontiguousarray(g["rwkv_ln_w"][L]),
        "ln_b": np.ascontiguousarray(g["rwkv_ln_b"][L]),
        "w_out": np.ascontiguousarray(g["w_out"][L]),
        "nw2T": np.ascontiguousarray(g["norm_ffn_w"][L].reshape(KD, 128).T),
        "w_query": np.ascontiguousarray(g["peer_w_query"][L]),
        "keysT": np.ascontiguousarray(g["peer_sub_keys"][L].reshape(16, 128, 128).transpose(2, 0, 1)),
        "uT": np.ascontiguousarray(g["peer_u"][L].reshape(128, 128, KD, 128).transpose(0, 3, 2, 1)),
        "v": np.ascontiguousarray(g["peer_v"][L]),
    }
    ar = np.arange(128)
    ident = np.eye(128, dtype=f32)
    tri = (ar[:, None] <= ar[None, :]).astype(f32)
    same = (ar[:, None] // 64 == ar[None, :] // 64)
    a64 = np.arange(64)
    mu_strict = (a64[None, :] > a64[:, None]).astype(f32)
    mu_incl = (a64[None, :] >= a64[:, None]).astype(f32)
    ml_strict = (a64[None, :] < a64[:, None]).astype(f32)
    cmask = np.stack([np.tile(m, (1, 8)) for m in (mu_strict, mu_incl, ml_strict)], axis=1)
    common.update({
        "ident": ident, "tri": tri, "ones": np.ones((128, 128), f32),
        "e0sel": (ar[:, None] == 0).astype(f32) * np.ones((1, 128), f32),
        "tri2": (tri * same).astype(f32), "blk2": same.astype(f32),
        "cmask": np.ascontiguousarray(cmask.astype(f32)),
        "idpat": ((ar[:, None] % 64) == a64[None, :]).astype(f32),
        "iota": np.tile(ar[None, :].astype(f32), (128, 1)),
        "blockmask": ((ar[:, None] // 16) == np.arange(8)[None, :]).astype(f32),
    })
    maps = []
    for core in range(8):
        b, s = core // 2, core % 2
        if s == 1:
            xc = np.ascontiguousarray(x[b])
        else:
            xc = np.concatenate([x[b, :OWN], x[b, :OWN]], axis=0)
        pf = np.ones((128, 1), f32)
        pf[0, 0] = float(s)
        m = dict(common)
        m.update({
            "xc": xc, "cT": np.ascontiguousarray(g["c"][b].reshape(KD, 128).T),
            "flagcol": np.full((128, 1), float(s), f32), "pflag": pf,
        })
        maps.append(m)
    return maps


def kernel(**inputs):
    nc = build()
    maps = host_inputs(inputs)
    maps = [{k: m[k] for k in nc._used_inputs} for m in maps]
    res = run_bass_kernel_spmd(nc, maps, core_ids=list(range(8)))
    out = np.zeros((4, 4096, D), np.float32)
    for core in range(8):
        b, s = core // 2, core % 2
        out[b, s * OWN:(s + 1) * OWN] = res.results[core]["y"]
    return out
```

```python
import numpy as np
from contextlib import ExitStack
import concourse.bass as bass
import concourse.mybir as mybir
from concourse.bass_utils import run_bass_kernel_spmd

F32 = mybir.dt.float32
BF16 = mybir.dt.bfloat16
U32 = mybir.dt.uint32
F32R = mybir.dt.float32r
AF = mybir.ActivationFunctionType
ALU = mybir.AluOpType
AX = mybir.AxisListType

D = 2048
KD = 16
CTX = 4096
OWN = 2048
IN_COLS = 7624
RW0 = 4104
RWC = 3520
NE = 16384
NORM_EPS = 1e-6
GN_EPS = 64e-5


class Sched:
    EPOCH = 30000

    def __init__(self, nc, es, same_engine_sync=True, n_dma_sems=8):
        self.nc = nc
        self.es = es
        self.engs = {'pe': nc.tensor, 'act': nc.scalar, 'dve': nc.vector, 'pool': nc.gpsimd, 'sp': nc.sync}
        self.cur = {}
        self.nsem = 0
        for e in ('pe', 'act', 'dve', 'pool'):
            self._new_epoch(e)
        self.waited = {e: {} for e in self.engs}
        self.res = {}
        self.same = same_engine_sync
        self.dma = {}
        for q in ('sp', 'act', 'pool'):
            sems = [self._mk(f"dma_{q}_{i}") for i in range(n_dma_sems)]
            self.dma[q] = {'sems': sems, 'cnt': [0] * n_dma_sems, 'next': 0}
        self.n_inst = {e: 0 for e in self.engs}

    def _mk(self, name):
        self.nsem += 1
        return self.es.enter_context(self.nc.semaphore(name))

    def _new_epoch(self, e):
        self.cur[e] = [self._mk(f"c_{e}_{self.nsem}"), 0]

    def _deps(self, reads, writes):
        evs = []
        for k in reads:
            r = self.res.get(k)
            if r and r['w'] is not None:
                evs.append(r['w'])
        for k in writes:
            r = self.res.get(k)
            if r:
                if r['w'] is not None:
                    evs.append(r['w'] + ('waw',))
                evs.extend(r['r'].values())
        return evs

    def _commit(self, ev, reads, writes):
        for k in reads:
            r = self.res.setdefault(k, {'w': None, 'r': {}})
            old = r['r'].get(id(ev[0]))
            if old is None or old[1] < ev[1]:
                r['r'][id(ev[0])] = ev
        for k in writes:
            self.res[k] = {'w': ev, 'r': {}}

    def _wait(self, e, evs):
        eng = self.engs[e]
        best = {}
        for ev in evs:
            sem, val, prod = ev[0], ev[1], ev[2]
            if prod == e and (e == 'pe' or not self.same or len(ev) > 3):
                continue
            ev = (sem, val, prod)
            if self.waited[e].get(id(sem), 0) >= val:
                continue
            if id(sem) not in best or best[id(sem)][1] < val:
                best[id(sem)] = ev
        for sem, val, prod in best.values():
            eng.wait_ge(sem, val)
            self.waited[e][id(sem)] = val

    def op(self, e, fn, reads=(), writes=()):
        writes = list(writes) + [k for k in reads if k.startswith('pb')]
        reads = [k for k in reads if not k.startswith('pb')]
        self._wait(e, self._deps(reads, writes))
        c = self.cur[e]
        if c[1] >= self.EPOCH:
            self._new_epoch(e)
            c = self.cur[e]
        inst = fn()
        c[1] += 1
        inst.then_inc(c[0], 1)
        ev = (c[0], c[1], e)
        self._commit(ev, reads, writes)
        self.n_inst[e] += 1
        return inst

    def dma_op(self, q, out, in_, reads=(), writes=()):
        d = self.dma[q]
        i = d['next']
        d['next'] = (i + 1) % len(d['sems'])
        sem = d['sems'][i]
        evs = self._deps(reads, writes)
        if d['cnt'][i] > 0:
            evs.append((sem, d['cnt'][i], 'dma'))
        self._wait(q, evs)
        inst = self.engs[q].dma_start(out=out, in_=in_)
        d['cnt'][i] += 16
        inst.then_inc(sem, 16)
        ev = (sem, d['cnt'][i], 'dma')
        self._commit(ev, reads, writes)
        self.n_inst[q] += 1
        return inst

    def barrier(self):
        evs = []
        for r in self.res.values():
            if r['w'] is not None:
                evs.append(r['w'])
            evs.extend(r['r'].values())
        for e in ('pe', 'act', 'dve', 'pool', 'sp'):
            self._wait(e, [(s_, v_, 'x') for (s_, v_, p_) in evs])
        self.res = {k: v for k, v in self.res.items() if k.endswith('_d') or k == 'y'}

    def finish(self):
        evs = []
        for r in self.res.values():
            if r['w'] is not None:
                evs.append(r['w'])
            evs.extend(r['r'].values())
        self._wait('sp', evs)


INPUT_SPECS = {
    "xc": ([CTX, D], F32), "cT": ([128, KD], F32), "w_ada": ([D, 6 * D], F32), "b_adaT": ([128, 96], F32),
    "nw1T": ([128, KD], F32), "w_in": ([D, IN_COLS], F32), "qnw": ([128, 1], F32), "knw": ([128, 1], F32),
    "fbias": ([8], F32), "mu": ([RWC], F32), "w0": ([1024], F32), "w_up": ([96, 1024], F32), "a0": ([1024], F32),
    "a_up": ([96, 1024], F32), "g_up": ([256, 1024], F32), "k_k": ([1024], F32), "k_a": ([1024], F32),
    "r_k": ([1024], F32), "ln_w": ([1024], F32), "ln_b": ([1024], F32), "w_out": ([D, D], F32),
    "nw2T": ([128, KD], F32), "w_query": ([D, D], F32), "keysT": ([128, 16, 128], F32),
    "uT": ([128, 128, KD, 128], F32), "v": ([NE, D], F32),
    "ident": ([128, 128], F32), "tri": ([128, 128], F32), "ones": ([128, 128], F32), "e0sel": ([128, 128], F32),
    "tri2": ([128, 128], F32), "blk2": ([128, 128], F32), "cmask": ([64, 3, 512], F32), "idpat": ([128, 64], F32),
    "pre_mod": ([128, 96], F32), "pre_f": ([128, 32, 8], F32),
    "iota": ([128, 128], F32), "blockmask": ([128, 8], F32), "flagcol": ([128, 1], F32), "pflag": ([128, 1], F32),
}


def build(stages=("A", "B", "C", "D", "E", "F"), debug=()):
    nc = bass.Bass("TRN2", target_bir_lowering=False)
    class _Lazy(dict):
        def __missing__(self, n):
            sh, dt = INPUT_SPECS[n]
            self[n] = nc.dram_tensor(n, sh, dt, kind="ExternalInput").ap()
            return self[n]
    I = _Lazy()
    nc._used_inputs = I
    y = nc.dram_tensor("y", [OWN, D], F32, kind="ExternalOutput").ap()
    dbg = {}
    for n, sh, dt in debug:
        dbg[n] = nc.dram_tensor(n, sh, dt, kind="ExternalOutput").ap()
    qT_d = nc.dram_tensor("qT_d", [8, 128, OWN], BF16).ap()
    kT_d = nc.dram_tensor("kT_d", [8, 128, CTX], BF16).ap()
    sg_d = nc.dram_tensor("sg_d", [8, 128, OWN], BF16).ap()
    v_d = nc.dram_tensor("v_d", [CTX, 1024], BF16).ap()
    rw_d = nc.dram_tensor("rw_d", [CTX + 1, RWC], F32).ap()
    mixT_d = nc.dram_tensor("mixT_d", [KD, 128, OWN], BF16).ap()
    x1_d = nc.dram_tensor("x1_d", [OWN, D], F32).ap()
    h2T_d = nc.dram_tensor("h2T_d", [128, KD, OWN], BF16).ap()
    uTb_d = nc.dram_tensor("uTb_d", [128, 128, KD * 128], BF16).ap()
    vb_d = nc.dram_tensor("vb_d", [NE, D], BF16).ap()
    qp_d = nc.dram_tensor("qp_d", [128, 16, OWN], F32).ap()

    with ExitStack() as es:
        S = Sched(nc, es)
        T, V_, A_, P_ = nc.tensor, nc.vector, nc.scalar, nc.gpsimd

        def sb(ctx, name, shape, dt):
            return ctx.enter_context(nc.sbuf_tensor("sb_" + name, shape, dt))

        banks = [es.enter_context(nc.psum_tensor(f"pb{i}", [128, 512], F32)) for i in range(8)]
        pk = [f"pb{i}" for i in range(8)]

        def mm(out, lhsT, rhs, start, stop, r, w):
            return S.op('pe', lambda: T.matmul(out, lhsT, rhs, start=start, stop=stop), reads=r, writes=w)

        def tr(out, in_, ident, r, w):
            return S.op('pe', lambda: T.transpose(out, in_, ident), reads=r, writes=w)

        def act(out, in_, func, r, w, **kw):
            return S.op('act', lambda: A_.activation(out=out, in_=in_, func=func, **kw), reads=r, writes=w)

        def ts(e, out, in0, s1, s2, op0, op1, r, w):
            eng = V_ if e == 'dve' else P_
            if op1 is None:
                return S.op(e, lambda: eng.tensor_scalar(out=out, in0=in0, scalar1=s1, scalar2=None, op0=op0), reads=r, writes=w)
            return S.op(e, lambda: eng.tensor_scalar(out=out, in0=in0, scalar1=s1, scalar2=s2, op0=op0, op1=op1), reads=r, writes=w)

        def tt(e, out, in0, in1, op, r, w):
            eng = V_ if e == 'dve' else P_
            return S.op(e, lambda: eng.tensor_tensor(out=out, in0=in0, in1=in1, op=op), reads=r, writes=w)

        def stt(out, in0, scalar, in1, op0, op1, r, w):
            return S.op('dve', lambda: V_.scalar_tensor_tensor(out=out, in0=in0, scalar=scalar, in1=in1, op0=op0, op1=op1), reads=r, writes=w)

        def cp(e, out, in_, r, w):
            if e == 'act':
                return S.op('act', lambda: A_.copy(out=out, in_=in_), reads=r, writes=w)
            eng = V_ if e == 'dve' else P_
            return S.op(e, lambda: eng.tensor_copy(out=out, in_=in_), reads=r, writes=w)

        def dma(q, out, in_, r, w):
            return S.dma_op(q, out, in_, reads=r, writes=w)

        ident = sb(es, "ident", [128, 128], F32)
        identb = sb(es, "identb", [128, 128], BF16)
        onesb = sb(es, "onesb", [128, 128], BF16)
        ones = sb(es, "ones", [128, 128], F32)
        mod = sb(es, "mod", [128, 96], F32)
        f_sb = sb(es, "f_sb", [128, 32, 8], F32)
        flagcol = sb(es, "flagcol", [128, 1], F32)
        pflag = sb(es, "pflag", [128, 1], F32)
        dma('sp', ident[:], I["ident"], [], ['ident'])
        dma('sp', ones[:], I["ones"], [], ['ones'])
        dma('sp', flagcol[:], I["flagcol"], [], ['flagcol'])
        dma('sp', pflag[:], I["pflag"], [], ['pflag'])
        cp('dve', identb[:], ident[:], ['ident'], ['identb'])
        cp('dve', onesb[:], ones[:], ['ones'], ['onesb'])

        pending_copies = []
        if "F" in stages:
            uTv = I["uT"].rearrange("i p k j -> i p (k j)")
            for c in range(16):
                pending_copies.append((uTb_d[c * 8:(c + 1) * 8], uTv[c * 8:(c + 1) * 8], 'uTb_d'))
                pending_copies.append((vb_d[c * 1024:(c + 1) * 1024, :], I["v"][c * 1024:(c + 1) * 1024, :], 'vb_d'))

        def issue_copies(n):
            for _ in range(n):
                if pending_copies:
                    o_, i_, k_ = pending_copies.pop(0)
                    dma('pool', o_, i_, [], [k_])

        if "A" in stages:
            with ExitStack() as ph:
                cs = sb(ph, "cs", [128, KD], F32)
                sc = sb(ph, "sc", [128, KD], F32)
                bT = sb(ph, "bT", [128, 96], F32)
                wa = [sb(ph, f"wa{i}", [128, KD, 768], F32) for i in range(2)]
                dma('sp', cs[:], I["cT"], [], ['cs'])
                dma('sp', bT[:], I["b_adaT"], [], ['bT'])
                act(sc[:], cs[:], AF.Silu, ['cs'], ['sc'])
                wv = I["w_ada"].rearrange("(k p) c -> p k c", p=128)
                for g in range(16):
                    wt = wa[g % 2]
                    wk = f"wa{g % 2}"
                    dma('sp', wt[:, 0:8, :], wv[:, 0:8, g * 768:(g + 1) * 768], [], [wk])
                    dma('sp', wt[:, 8:16, :], wv[:, 8:16, g * 768:(g + 1) * 768], [], [wk])
                    for ct in range(6):
                        n = g * 6 + ct
                        for k in range(KD):
                            mm(banks[0][:, n:n + 1], wt[:, k, ct * 128:(ct + 1) * 128], sc[:, k:k + 1],
                               k == 0, k == KD - 1, [wk, 'sc'], [pk[0]])
                tt('dve', mod[:], banks[0][:, 0:96], bT[:], ALU.add, [pk[0], 'bT'], ['mod'])
        S.barrier()
        if "modT" in dbg:
            dma('sp', dbg["modT"], mod[:], ['mod'], ['dbg_modT'])

        if "A" not in stages:
            dma('sp', mod[:], I["pre_mod"], [], ['mod'])
        if "B" not in stages and "C" in stages:
            dma('sp', f_sb[:], I["pre_f"], [], ['f_sb'])
        sh1, sc1, g1c, sh2, sc2, g2c = [mod[:, i * 16:(i + 1) * 16] for i in range(6)]

        if "B" in stages:
            with ExitStack() as ph:
                hT = sb(ph, "hT", [128, KD, OWN], BF16)
                xt = [sb(ph, f"xt{i}", [128, D], F32) for i in range(2)]
                junk = sb(ph, "junk", [128, D], BF16)
                xn = sb(ph, "xn", [128, D], BF16)
                ss = sb(ph, "ss", [128, 32], F32)
                rstd = sb(ph, "rstd", [128, 32], F32)
                scl1 = sb(ph, "scl1", [128, KD], F32)
                nw1 = sb(ph, "nw1", [128, KD], F32)
                wg = [sb(ph, f"wg{i}", [128, KD, 512], BF16) for i in range(2)]
                wf = sb(ph, "wf", [128, KD, 8], BF16)
                stg = [sb(ph, f"stg{i}", [128, 512], F32) for i in range(2)]
                stgb = [sb(ph, f"stgb{i}", [128, 512], BF16) for i in range(2)]
                raw = sb(ph, "raw", [128, 512], F32)
                sq = sb(ph, "sq", [128, 512], BF16)
                lnt = sb(ph, "lnt", [128, 512], F32)
                rs = sb(ph, "rs", [128, 512], F32)
                qw = sb(ph, "qw", [128, 1], F32)
                kw_ = sb(ph, "kw_", [128, 1], F32)
                fb = sb(ph, "fb", [128, 8], F32)
                zrow = sb(ph, "zrow", [1, RWC], F32)
                epsc = sb(ph, "epsc", [128, 1], F32)
                S.op('dve', lambda: V_.memset(zrow[:], 0.0), [], ['zrow'])
                S.op('dve', lambda: V_.memset(epsc[:], NORM_EPS), [], ['epsc'])
                dma('sp', rw_d[0:1, :], zrow[:], ['zrow'], ['rw_d'])
                dma('sp', nw1[:], I["nw1T"], [], ['nw1'])
                dma('sp', qw[:], I["qnw"], [], ['qw'])
                dma('sp', kw_[:], I["knw"], [], ['kw_'])
                dma('sp', fb[:], I["fbias"].partition_broadcast(128), [], ['fb'])
                stt(scl1[:], sc1, 1.0, nw1[:], ALU.add, ALU.mult, ['mod', 'nw1'], ['scl1'])
                ts('dve', qw[:], qw[:], 128.0 ** -0.5, None, ALU.mult, None, ['qw'], ['qw'])
                wv = I["w_in"].rearrange("(k p) c -> p k c", p=128)
                dma('pool', wf[:], wv[:, :, 4096:4104], [], ['wf'])
                gi = 0
                obank = 0
                for th in range(2):
                    for t16 in range(16):
                        tile = th * 16 + t16
                        xb_ = xt[tile % 2]
                        xk = f"xt{tile % 2}"
                        dma('sp', xb_[:], I["xc"][tile * 128:(tile + 1) * 128, :], [], [xk])
                        act(junk[:], xb_[:], AF.Square, [xk], ['junk'], accum_out=ss[:, tile:tile + 1])
                        ts('dve', rstd[:, tile:tile + 1], ss[:, tile:tile + 1], 1.0 / D, NORM_EPS, ALU.mult, ALU.add, ['junk'], ['rstd'])
                        act(rstd[:, tile:tile + 1], rstd[:, tile:tile + 1], AF.Sqrt, ['rstd'], ['rstd'])
                        S.op('dve', lambda: V_.reciprocal(out=rstd[:, tile:tile + 1], in_=rstd[:, tile:tile + 1]), ['rstd'], ['rstd'])
                        act(xn[:], xb_[:], AF.Copy, [xk, 'rstd'], ['xn'], scale=rstd[:, tile:tile + 1])
                        for half in range(2):
                            pbv = banks[half][:].bitcast(BF16)
                            for kk in range(8):
                                k = half * 8 + kk
                                tr(pbv[:, kk * 128:(kk + 1) * 128], xn[:, k * 128:(k + 1) * 128], identb[:], ['xn', 'identb'], [pk[half]])
                            for kk in range(8):
                                k = half * 8 + kk
                                o = hT[:, k, t16 * 128:(t16 + 1) * 128]
                                if kk % 2 == 0:
                                    ts('dve', o, pbv[:, kk * 128:(kk + 1) * 128], scl1[:, k:k + 1], sh1[:, k:k + 1], ALU.mult, ALU.add,
                                       [pk[half], 'scl1', 'mod'], ['hT'])
                                else:
                                    act(o, pbv[:, kk * 128:(kk + 1) * 128], AF.Identity, [pk[half], 'scl1', 'mod'], ['hT'],
                                        scale=scl1[:, k:k + 1], bias=sh1[:, k:k + 1])
                    groups = []
                    if th == 1:
                        groups += [("q", 0, 512), ("q", 512, 512)]
                    groups += [("k", 1024, 512), ("k", 1536, 512), ("v", 2048, 512), ("v", 2560, 512)]
                    if th == 1:
                        groups += [("g", 3072, 512), ("g", 3584, 512)]
                    c0 = RW0
                    while c0 < IN_COLS:
                        n = min(512, IN_COLS - c0)
                        groups.append(("r", c0, n))
                        c0 += n
                    for (kind, c0, n) in groups:
                        wt = wg[gi % 2]
                        wk = f"wg{gi % 2}"
                        gi += 1
                        dma('pool', wt[:, :, 0:n], wv[:, :, c0:c0 + n], [], [wk])
                        issue_copies(2)
                        if kind in ("q", "k", "g"):
                            for tb in range(4):
                                for hh in range(4):
                                    head = (c0 % 1024) // 128 + hh
                                    bk = 2 + obank % 4
                                    obank += 1
                                    for k in range(KD):
                                        mm(banks[bk][:], wt[:, k, hh * 128:(hh + 1) * 128], hT[:, k, tb * 512:(tb + 1) * 512],
                                           k == 0, k == KD - 1, [wk, 'hT'], [pk[bk]])
                                    so = stgb[obank % 2]
                                    sk = f"stgb{obank % 2}"
                                    if kind == "g":
                                        act(so[:], banks[bk][:], AF.Sigmoid, [pk[bk]], [sk])
                                        dma('sp', sg_d[head, :, tb * 512:(tb + 1) * 512], so[:], [sk], ['sg_d'])
                                    else:
                                        cp('dve', raw[:], banks[bk][:], [pk[bk]], ['raw'])
                                        act(sq[:], banks[bk][:], AF.Square, [pk[bk]], ['sq'])
                                        mm(banks[6][:], onesb[:], sq[:], True, True, ['onesb', 'sq'], [pk[6]])
                                        act(lnt[:], banks[6][:], AF.Ln, [pk[6], 'epsc'], ['lnt'], scale=1.0 / 128, bias=epsc[:])
                                        act(rs[:], lnt[:], AF.Exp, ['lnt'], ['rs'], scale=-0.5)
                                        wcol = qw if kind == "q" else kw_
                                        stt(so[:], raw[:], wcol[:, 0:1], rs[:], ALU.mult, ALU.mult, ['raw', 'rs', 'qw', 'kw_'], [sk])
                                        if kind == "q":
                                            dma('sp', qT_d[head, :, tb * 512:(tb + 1) * 512], so[:], [sk], ['qT_d'])
                                        else:
                                            dma('sp', kT_d[head, :, th * OWN + tb * 512: th * OWN + (tb + 1) * 512], so[:], [sk], ['kT_d'])
                        else:
                            for t16 in range(16):
                                tile = th * 16 + t16
                                bk = 2 + obank % 4
                                obank += 1
                                for k in range(KD):
                                    mm(banks[bk][:, 0:n], hT[:, k, t16 * 128:(t16 + 1) * 128], wt[:, k, 0:n],
                                       k == 0, k == KD - 1, [wk, 'hT'], [pk[bk]])
                                if kind == "v":
                                    so = stgb[obank % 2]
                                    sk = f"stgb{obank % 2}"
                                    if obank % 2:
                                        act(so[:, 0:n], banks[bk][:, 0:n], AF.Copy, [pk[bk]], [sk])
                                    else:
                                        cp('dve', so[:, 0:n], banks[bk][:, 0:n], [pk[bk]], [sk])
                                    dma('sp', v_d[tile * 128:(tile + 1) * 128, c0 - 2048:c0 - 2048 + n], so[:, 0:n], [sk], ['v_d'])
                                else:
                                    so = stg[obank % 2]
                                    sk = f"stg{obank % 2}"
                                    if obank % 2:
                                        act(so[:, 0:n], banks[bk][:, 0:n], AF.Copy, [pk[bk]], [sk])
                                    else:
                                        cp('dve', so[:, 0:n], banks[bk][:, 0:n], [pk[bk]], [sk])
                                    dma('sp', rw_d[1 + tile * 128:1 + (tile + 1) * 128, c0 - RW0:c0 - RW0 + n], so[:, 0:n], [sk], ['rw_d'])
                    for t16 in range(16):
                        tile = th * 16 + t16
                        for k in range(KD):
                            mm(banks[7][:, 0:8], hT[:, k, t16 * 128:(t16 + 1) * 128], wf[:, k, :], k == 0, k == KD - 1, ['wf', 'hT'], [pk[7]])
                        tt('dve', f_sb[:, tile, :], banks[7][:, 0:8], fb[:], ALU.add, [pk[7], 'fb'], ['f_sb'])
        S.barrier()
        for n in ("qT_d", "kT_d", "sg_d", "v_d", "rw_d"):
            if "dbg_" + n in dbg:
                src = {"qT_d": qT_d, "kT_d": kT_d, "sg_d": sg_d, "v_d": v_d, "rw_d": rw_d}[n]
                dma('sp', dbg["dbg_" + n], src, [n], ["dbg_" + n])
        if "dbg_f" in dbg:
            dma('sp', dbg["dbg_f"], f_sb[:], ['f_sb'], ['dbg_f'])

        issue_copies(len(pending_copies))
        if "C" in stages:
            with ExitStack() as ph:
                tri = sb(ph, "tri", [128, 128], F32)
                trib = sb(ph, "trib", [128, 128], BF16)
                e0 = sb(ph, "e0", [128, 128], F32)
                lf = sb(ph, "lf", [128, 256], F32)
                cum = sb(ph, "cum", [128, 256], F32)
                tot = sb(ph, "tot", [128, 256], F32)
                off = sb(ph, "off", [128, 256], F32)
                ncb = sb(ph, "ncb", [128, 256], F32)
                cref = sb(ph, "cref", [128, 256], F32)
                kbcol = sb(ph, "kbcol", [128, 1], F32)
                onec = sb(ph, "onec", [128, 1], F32)
                biasall = sb(ph, "biasall", [128, 8, 16, 32], F32)
                kT_h = sb(ph, "kT_h", [128, CTX], BF16)
                qT_h = sb(ph, "qT_h", [128, OWN], BF16)
                v_h = sb(ph, "v_h", [128, 32, 128], BF16)
                sg_h = sb(ph, "sg_h", [128, OWN], BF16)
                pT = [sb(ph, f"pT{i}", [128, 512], BF16) for i in range(2)]
                rec = sb(ph, "rec", [128, 512], F32)
                o1 = sb(ph, "o1", [128, 512], F32)
                o2 = [sb(ph, f"o2{i}", [128, 512], BF16) for i in range(2)]
                dma('sp', tri[:], I["tri"], [], ['tri'])
                dma('sp', e0[:], I["e0sel"], [], ['e0'])
                cp('dve', trib[:], tri[:], ['tri'], ['trib'])
                S.op('dve', lambda: V_.memset(onec[:], 1.0), [], ['onec'])
                fflat = f_sb[:].rearrange("p c h -> p (c h)")
                act(lf[:], fflat, AF.Exp, ['f_sb'], ['lf'], scale=-1.0)
                act(lf[:], lf[:], AF.Ln, ['lf', 'onec'], ['lf'], bias=onec[:])
                ts('dve', lf[:], lf[:], -1.0, None, ALU.mult, None, ['lf'], ['lf'])
                mm(banks[0][:, 0:256], tri[:], lf[:], True, True, ['tri', 'lf'], [pk[0]])
                mm(banks[1][:, 0:256], ones[:], lf[:], True, True, ['ones', 'lf'], [pk[1]])
                cp('dve', tot[:], banks[1][:, 0:256], [pk[1]], ['tot'])
                S.op('dve', lambda: V_.memset(off[:], 0.0), [], ['off'])
                for c in range(1, 32):
                    tt('dve', off[:, c * 8:(c + 1) * 8], off[:, (c - 1) * 8:c * 8], tot[:, (c - 1) * 8:c * 8], ALU.add, ['off', 'tot'], ['off'])
                tt('dve', cum[:], banks[0][:, 0:256], off[:], ALU.add, [pk[0], 'off'], ['cum'])
                mm(banks[2][:, 0:256], e0[:], cum[:], True, True, ['e0', 'cum'], [pk[2]])
                cp('dve', cref[:], banks[2][:, 0:256], [pk[2]], ['cref'])
                ts('dve', kbcol[:], flagcol[:], -1.0, 30000.0, ALU.add, ALU.mult, ['flagcol'], ['kbcol'])
                ts('dve', ncb[:], cum[:], -1.0, None, ALU.mult, None, ['cum'], ['ncb'])
                ts('dve', ncb[:, 0:128], ncb[:, 0:128], kbcol[:, 0:1], None, ALU.add, None, ['ncb', 'kbcol'], ['ncb'])
                ncb3 = ncb[:].rearrange("p (c h) -> p c h", h=8)
                for h in range(8):
                    for qi in range(16):
                        col = (16 + qi) * 8 + h
                        ts('dve', biasall[:, h, qi, :], ncb3[:, :, h], cref[:, col:col + 1], None, ALU.add, None, ['ncb', 'cref'], ['biasall'])
                vview = v_d.rearrange("(c p) f -> p c f", p=128)
                ob = 0
                for h in range(8):
                    dma('sp', kT_h[:], kT_d[h], ['kT_d'], ['kT_h'])
                    dma('sp', qT_h[:], qT_d[h], ['qT_d'], ['qT_h'])
                    dma('sp', v_h[:], vview[:, :, h * 128:(h + 1) * 128], ['v_d'], ['v_h'])
                    dma('sp', sg_h[:], sg_d[h], ['sg_d'], ['sg_h'])
                    for qb in range(4):
                        nk = 16 + 4 * qb + 4
                        bO = banks[2 + 2 * (ob % 2)]
                        kO = pk[2 + 2 * (ob % 2)]
                        bR = banks[3 + 2 * (ob % 2)]
                        kR = pk[3 + 2 * (ob % 2)]
                        def fox_qk(kt):
                            c0 = max(0, kt - (16 + 4 * qb)) * 128
                            mm(banks[kt % 2][:, c0:512], kT_h[:, kt * 128:(kt + 1) * 128], qT_h[:, qb * 512 + c0:(qb + 1) * 512], True, True,
                               ['kT_h', 'qT_h'], [pk[kt % 2]])

                        def fox_act(kt):
                            jmin = max(0, kt - (16 + 4 * qb))
                            sbk = banks[kt % 2]
                            sk = pk[kt % 2]
                            pt = pT[kt % 2]
                            ptk = f"pT{kt % 2}"
                            for j in range(jmin, 4):
                                qi = 4 * qb + j
                                act(pt[:, j * 128:(j + 1) * 128], sbk[:, j * 128:(j + 1) * 128], AF.Exp, [sk, 'biasall'], [ptk],
                                    bias=biasall[:, h, qi, kt:kt + 1], scale=1.0)
                                if kt == 16 + qi:
                                    tt('dve', pt[:, j * 128:(j + 1) * 128], pt[:, j * 128:(j + 1) * 128], trib[:], ALU.mult, [ptk, 'trib'], [ptk])

                        def fox_pv(kt):
                            c0 = max(0, kt - (16 + 4 * qb)) * 128
                            pt = pT[kt % 2]
                            ptk = f"pT{kt % 2}"
                            mm(bO[:, c0:512], v_h[:, kt, :], pt[:, c0:512], kt == 0, kt == nk - 1, ['v_h', ptk], [kO])
                            mm(bR[:, c0:512], onesb[:], pt[:, c0:512], kt == 0, kt == nk - 1, ['onesb', ptk], [kR])

                        fox_qk(0)
                        for kt in range(nk):
                            fox_act(kt)
                            if kt + 1 < nk:
                                fox_qk(kt + 1)
                            fox_pv(kt)
                        S.op('dve', lambda: V_.reciprocal(out=rec[:], in_=bR[:]), [kR], ['rec'])
                        tt('dve', o1[:], bO[:], rec[:], ALU.mult, [kO, 'rec'], ['o1'])
                        oo = o2[ob % 2]
                        ok_ = f"o2{ob % 2}"
                        tt('pool', oo[:], o1[:], sg_h[:, qb * 512:(qb + 1) * 512], ALU.mult, ['o1', 'sg_h'], [ok_])
                        dma('sp', mixT_d[h, :, qb * 512:(qb + 1) * 512], oo[:], [ok_], ['mixT_d'])
                        ob += 1
            S.barrier()
        if "D" in stages:
            with ExitStack() as ph:
                mub = sb(ph, "mub", [128, RWC], F32)
                cvec = {}
                for nm in ("w0", "a0", "k_k", "k_a", "r_k", "ln_w", "ln_b"):
                    cvec[nm] = sb(ph, "c_" + nm, [128, 1024], F32)
                    dma('sp', cvec[nm][:], I[nm].partition_broadcast(128), [], ['c_' + nm])
                dma('sp', mub[:], I["mu"].partition_broadcast(128), [], ['mub'])
                wup = sb(ph, "wup", [96, 1024], F32)
                aup = sb(ph, "aup", [96, 1024], F32)
                gup = sb(ph, "gup", [128, 2, 1024], F32)
                dma('sp', wup[:], I["w_up"], [], ['wup'])
                dma('sp', aup[:], I["a_up"], [], ['aup'])
                dma('sp', gup[:], I["g_up"].rearrange("(k p) c -> p k c", p=128), [], ['gup'])
                tri2 = sb(ph, "tri2", [128, 128], F32)
                blk2 = sb(ph, "blk2", [128, 128], F32)
                idp = sb(ph, "idp", [128, 64], F32)
                cmf = sb(ph, "cmf", [64, 3, 512], F32)
                cmb = sb(ph, "cmb", [64, 3, 512], BF16)
                irep = sb(ph, "irep", [64, 8, 64], BF16)
                dma('sp', tri2[:], I["tri2"], [], ['tri2'])
                dma('sp', blk2[:], I["blk2"], [], ['blk2'])
                dma('sp', idp[:], I["idpat"], [], ['idp'])
                dma('sp', cmf[:], I["cmask"], [], ['cmf'])
                cp('dve', cmb[:], cmf[:], ['cmf'], ['cmb'])
                cp('dve', irep[:], idp[0:64, :].unsqueeze(1).to_broadcast([64, 8, 64]), ['idp'], ['irep'])
                MU_S, MU_I, ML_S = cmb[:, 0, :], cmb[:, 1, :], cmb[:, 2, :]
                i64b = identb[0:64, 0:64]
                Rb = sb(ph, "Rb", [128, 1024], F32)
                Kb = sb(ph, "Kb", [128, 1024], F32)
                Vb = sb(ph, "Vb", [128, 1024], F32)
                LO = sb(ph, "LO", [128, 448], F32)
                A1 = sb(ph, "A1", [128, 1024], F32)
                A2 = sb(ph, "A2", [128, 1024], F32)
                A3 = sb(ph, "A3", [128, 1024], F32)
                A4 = sb(ph, "A4", [128, 1024], F32)
                A5 = sb(ph, "A5", [128, 1024], F32)
                A6 = sb(ph, "A6", [128, 1024], F32)
                TMP = sb(ph, "TMP", [128, 1024], F32)
                st16 = sb(ph, "st16", [128, 16], F32)
                st16b = sb(ph, "st16b", [128, 16], F32)
                lt = sb(ph, "lt", [128, 448], F32)
                ltT = sb(ph, "ltT", [128, 4, 128], F32)
                tm = {}
                for nm in ("rh", "ah", "bh", "kh", "bb", "kb", "vb", "Fm"):
                    tm[nm] = sb(ph, "t_" + nm, [128, 1024], BF16)
                sh = {}
                for nm in ("ah", "vb", "bb", "kb", "Fm"):
                    sh[nm] = sb(ph, "s_" + nm, [64, 1024], BF16)
                FM = sb(ph, "FM", [64, 16, 4, 128], BF16)
                irepf = sb(ph, "irepf", [64, 8, 64], F32)
                cp('dve', irepf[:], idp[0:64, :].unsqueeze(1).to_broadcast([64, 8, 64]), ['idp'], ['irepf'])
                onesw = sb(ph, "onesw", [64, 512], F32)
                identr = sb(ph, "identr", [64, 64], F32)
                S.op('dve', lambda: V_.memset(onesw[:], 1.0), [], ['onesw'])
                tt('dve', identr[:].bitcast(F32R), ident[0:64, 0:64], onesw[:, 0:64], ALU.mult, ['ident', 'onesw'], ['identr'])
                CB = [{}, {}]
                CB[0]['Lb'] = [(sb(ph, f"Lb{i}", [64, 8, 64], F32)[:], f"Lb{i}") for i in range(2)]
                CB[0]['Lpb'] = [(sb(ph, f"Lpb{i}", [64, 8, 64], F32)[:], f"Lpb{i}") for i in range(2)]
                CB[0]['TTb'] = [(sb(ph, f"TTb{i}", [64, 8, 64], F32)[:], f"TTb{i}") for i in range(2)]
                for nm in ('TTfb', 'MakT', 'MrbT', 'MrkT', 'Wb', 'Atb', 'Vtb', 'RtT', 'Pmb'):
                    CB[0][nm] = (sb(ph, nm, [64, 8, 64], BF16)[:], nm)

                def v32(buf, half):
                    return buf[0:64, half * 512:(half + 1) * 512].rearrange("p (h c) -> p h c", c=64)

                def v16(buf, q):
                    return buf[:].bitcast(BF16)[0:64, q * 512:(q + 1) * 512].rearrange("p (h c) -> p h c", c=64)

                CB[1]['Lb'] = [(sb(ph, f"Lb1_{i}", [64, 8, 64], F32)[:], f"Lb1_{i}") for i in range(2)]
                CB[1]['Lpb'] = [(sb(ph, f"Lpb1_{i}", [64, 8, 64], F32)[:], f"Lpb1_{i}") for i in range(2)]
                CB[1]['TTb'] = [(sb(ph, f"TTb1_{i}", [64, 8, 64], F32)[:], f"TTb1_{i}") for i in range(2)]
                for q_, nm in enumerate(('TTfb', 'MakT', 'MrbT', 'MrkT')):
                    CB[1][nm] = (v16(A6, q_), 'A6')
                for q_, nm in enumerate(('Wb', 'Atb', 'Vtb', 'RtT')):
                    CB[1][nm] = (v16(Kb, q_), 'Kb')
                CB[1]['Pmb'] = (sb(ph, "Pmb1", [64, 8, 64], BF16)[:], 'Pmb1')
                Zb = sb(ph, "Zb", [64, 16, 64], BF16)
                Yt = sb(ph, "Yt", [128, 1024], F32)
                PRV = Yt
                Ych = TMP[0:64, :]
                yb = sb(ph, "yb", [128, 1024], BF16)
                mst = sb(ph, "mst", [128, 8, 128], BF16)
                S.op('dve', lambda: V_.memset(Zb[:], 0.0), [], ['Zb'])
                bank_ctr = [0]

                def nbk():
                    b = bank_ctr[0] % 8
                    bank_ctr[0] += 1
                    return b

                def h3(t_, w=64):
                    return t_.rearrange("p (h c) -> p h c", c=w)

                def bc16(t16_):
                    return t16_.unsqueeze(2).to_broadcast([128, 16, 64])

                for tile in range(32):
                    own = tile >= 16
                    r0 = tile * 128
                    for si_, (buf, key, c0, n) in enumerate(((Rb, 'Rb', 0, 1024), (Kb, 'Kb', 1024, 1024), (Vb, 'Vb', 2048, 1024), (LO, 'LO', 3072, 448))):
                        pv, pvk = (Yt, 'Yt') if si_ % 2 == 0 else (TMP, 'TMP')
                        dma('sp', buf[:, 0:n], rw_d[1 + r0:1 + r0 + 128, c0:c0 + n], ['rw_d'], [key])
                        dma('sp', pv[:, 0:n], rw_d[r0:r0 + 128, c0:c0 + n], ['rw_d'], [pvk])
                        if tile == 16:
                            ts('dve', pv[:, 0:n], pv[:, 0:n], pflag[:, 0:1], None, ALU.mult, None, [pvk, 'pflag'], [pvk])
                        tt('pool', pv[:, 0:n], pv[:, 0:n], buf[:, 0:n], ALU.subtract, [pvk, key], [pvk])
                        tt('pool', pv[:, 0:n], pv[:, 0:n], mub[:, c0:c0 + n], ALU.mult, [pvk, 'mub'], [pvk])
                        tt('dve', buf[:, 0:n], buf[:, 0:n], pv[:, 0:n], ALU.add, [key, pvk], [key])
                    act(lt[:, 0:96], LO[:, 0:96], AF.Tanh, ['LO'], ['lt'])
                    cp('dve', lt[:, 96:192], LO[:, 96:192], ['LO'], ['lt'])
                    act(lt[:, 192:448], LO[:, 192:448], AF.Sigmoid, ['LO'], ['lt'])
                    bT_ = nbk()
                    tr(banks[bT_][0:96, 0:128], lt[:, 0:96], ident[:], ['lt', 'ident'], [pk[bT_]])
                    tr(banks[bT_][0:96, 128:256], lt[:, 96:192], ident[:], ['lt', 'ident'], [pk[bT_]])
                    tr(banks[bT_][:, 256:384], lt[:, 192:320], ident[:], ['lt', 'ident'], [pk[bT_]])
                    tr(banks[bT_][:, 384:512], lt[:, 320:448], ident[:], ['lt', 'ident'], [pk[bT_]])
                    cp('dve', ltT[0:96, 0:2, :].rearrange("p a t -> p (a t)"), banks[bT_][0:96, 0:256], [pk[bT_]], ['ltT'])
                    cp('dve', ltT[:, 2:4, :].rearrange("p a t -> p (a t)"), banks[bT_][:, 256:512], [pk[bT_]], ['ltT'])
                    for half in range(2):
                        b_ = nbk()
                        mm(banks[b_][:], ltT[0:96, 0, :], wup[:, half * 512:(half + 1) * 512], True, True, ['ltT', 'wup'], [pk[b_]])
                        tt('dve', A1[:, half * 512:(half + 1) * 512], banks[b_][:], cvec["w0"][:, half * 512:(half + 1) * 512], ALU.add, [pk[b_], 'c_w0'], ['A1'])
                        b_ = nbk()
                        mm(banks[b_][:], ltT[0:96, 1, :], aup[:, half * 512:(half + 1) * 512], True, True, ['ltT', 'aup'], [pk[b_]])
                        tt('dve', A2[:, half * 512:(half + 1) * 512], banks[b_][:], cvec["a0"][:, half * 512:(half + 1) * 512], ALU.add, [pk[b_], 'c_a0'], ['A2'])
                        b_ = nbk()
                        mm(banks[b_][:], ltT[:, 2, :], gup[:, 0, half * 512:(half + 1) * 512], True, False, ['ltT', 'gup'], [pk[b_]])
                        mm(banks[b_][:], ltT[:, 3, :], gup[:, 1, half * 512:(half + 1) * 512], False, True, ['ltT', 'gup'], [pk[b_]])
                        cp('act', A3[:, half * 512:(half + 1) * 512], banks[b_][:], [pk[b_]], ['A3'])
                    act(A1[:], A1[:], AF.Sigmoid, ['A1'], ['A1'])
                    ts('pool', A1[:], A1[:], -0.6065306597126334, None, ALU.mult, None, ['A1'], ['A1'])
                    act(A2[:], A2[:], AF.Sigmoid, ['A2'], ['A2'])
                    tt('dve', A4[:], Kb[:], cvec["k_k"][:], ALU.mult, ['Kb', 'c_k_k'], ['A4'])
                    tt('pool', TMP[:], A4[:], A4[:], ALU.mult, ['A4'], ['TMP'])
                    S.op('dve', lambda: V_.tensor_reduce(out=st16[:], in_=h3(TMP[:]), axis=AX.X, op=ALU.add), ['TMP'], ['st16'])
                    act(st16[:], st16[:], AF.Sqrt, ['st16'], ['st16'])
                    ts('dve', st16[:], st16[:], 1e-12, None, ALU.max, None, ['st16'], ['st16'])
                    S.op('dve', lambda: V_.reciprocal(out=st16[:], in_=st16[:]), ['st16'], ['st16'])
                    tt('dve', h3(A4[:]), h3(A4[:]), bc16(st16[:]), ALU.mult, ['A4', 'st16'], ['A4'])
                    stt(TMP[:], A2[:], -1.0, cvec["k_a"][:], ALU.add, ALU.mult, ['A2', 'c_k_a'], ['TMP'])
                    stt(A5[:], TMP[:], 1.0, Kb[:], ALU.add, ALU.mult, ['TMP', 'Kb'], ['A5'])
                    tt('pool', A6[:], A4[:], A2[:], ALU.mult, ['A4', 'A2'], ['A6'])
                    bcw = [nbk(), nbk()]
                    bcc = [nbk(), nbk()]
                    for half in range(2):
                        mm(banks[bcw[half]][:], tri2[:], A1[:, half * 512:(half + 1) * 512], True, True, ['tri2', 'A1'], [pk[bcw[half]]])
                        mm(banks[bcc[half]][:], blk2[:], A1[:, half * 512:(half + 1) * 512], True, True, ['blk2', 'A1'], [pk[bcc[half]]])
                    for half in range(2):
                        sl = slice(half * 512, (half + 1) * 512)
                        cw = banks[bcw[half]][:]
                        cc = banks[bcc[half]][:]
                        kw_ = pk[bcw[half]]
                        kc_ = pk[bcc[half]]
                        act(TMP[:, sl], cw, AF.Exp, [kw_], ['TMP'])
                        tt('dve', tm["rh"][:, sl], Rb[:, sl], TMP[:, sl], ALU.mult, ['Rb', 'TMP'], ['t_rh'])
                        act(TMP[:, sl], cw, AF.Exp, [kw_], ['TMP'], scale=-1.0)
                        tt('dve', tm["bh"][:, sl], A6[:, sl], TMP[:, sl], ALU.mult, ['A6', 'TMP'], ['t_bh'])
                        tt('pool', tm["kh"][:, sl], A5[:, sl], TMP[:, sl], ALU.mult, ['A5', 'TMP'], ['t_kh'])
                        tt('dve', TMP[:, sl], cw, A1[:, sl], ALU.subtract, [kw_, 'A1'], ['TMP'])
                        act(TMP[:, sl], TMP[:, sl], AF.Exp, ['TMP'], ['TMP'])
                        stt(tm["ah"][:, sl], A4[:, sl], -1.0, TMP[:, sl], ALU.mult, ALU.mult, ['A4', 'TMP'], ['t_ah'])
                        cp('act', Kb[:, sl], cw, [kw_], ['Kb'])
                        tt('dve', TMP[:, sl], cc, Kb[:, sl], ALU.subtract, [kc_, 'Kb'], ['TMP'])
                        act(TMP[:, sl], TMP[:, sl], AF.Exp, ['TMP'], ['TMP'])
                        tt('dve', tm["bb"][:, sl], A6[:, sl], TMP[:, sl], ALU.mult, ['A6', 'TMP'], ['t_bb'])
                        tt('pool', tm["kb"][:, sl], A5[:, sl], TMP[:, sl], ALU.mult, ['A5', 'TMP'], ['t_kb'])
                        act(TMP[:, sl], cc, AF.Exp, [kc_], ['TMP'])
                        tt('dve', h3(tm["Fm"][:, sl]), h3(TMP[:, sl]), idp[:].unsqueeze(1).to_broadcast([128, 8, 64]), ALU.mult, ['TMP', 'idp'], ['t_Fm'])
                    cp('pool', tm["vb"][:], Vb[:], ['Vb'], ['t_vb'])
                    for nm in ("ah", "vb", "bb", "kb", "Fm"):
                        dma('sp', sh[nm][:], tm[nm][64:128, :], ['t_' + nm], ['s_' + nm])
                    for xi, nm in enumerate(("rh", "ah", "bh", "kh")):
                        if nm == "rh" and not own:
                            continue
                        for hg in range(2):
                            b_ = nbk()
                            pbv = banks[b_][:].bitcast(BF16)
                            for hh in range(8):
                                h = hg * 8 + hh
                                tr(pbv[0:64, hh * 128:(hh + 1) * 128], tm[nm][:, h * 64:(h + 1) * 64], identb[:], ['t_' + nm, 'identb'], [pk[b_]])
                            src = pbv[0:64, :].rearrange("p (h t) -> p h t", t=128)
                            if (xi + hg) % 2:
                                act(FM[:, hg * 8:(hg + 1) * 8, xi, :], src, AF.Copy, [pk[b_]], ['FM'])
                            else:
                                cp('dve', FM[:, hg * 8:(hg + 1) * 8, xi, :], src, [pk[b_]], ['FM'])
                    for c in range(2):
                        cs = slice(c * 64, (c + 1) * 64)
                        gchunk = tile * 2 + c
                        if gchunk == 32:
                            ts('dve', Zb[:], Zb[:], flagcol[0:64, 0:1], None, ALU.mult, None, ['Zb', 'flagcol'], ['Zb'])

                        def chunk_steps(hg, c=c, cs=cs):
                            B = CB[hg]
                            hs = [hg * 8 + hh for hh in range(8)]
                            evi = [hg]

                            def tmop(nm, h):
                                if c == 0:
                                    return tm[nm][0:64, h * 64:(h + 1) * 64], 't_' + nm
                                return sh[nm][:, h * 64:(h + 1) * 64], 's_' + nm

                            def evac(dst, in_, key_in, mask=None):
                                out, key_out = dst
                                evi[0] += 1
                                o2 = out.rearrange("p h c -> p (h c)")
                                if mask is not None:
                                    tt('dve', o2, in_, mask, ALU.mult, [key_in, 'cmb'], [key_out])
                                elif evi[0] % 2:
                                    act(o2, in_, AF.Copy, [key_in], [key_out])
                                else:
                                    cp('dve', o2, in_, [key_in], [key_out])

                            def fmT(xi, h):
                                return FM[:, h, xi, cs]

                            def R_(ap):
                                return ap.bitcast(F32R)

                            def evacr(dst, in_, key_in):
                                out, key_out = dst
                                evi[0] += 1
                                o2 = R_(out).rearrange("p h c -> p (h c)")
                                if evi[0] % 2:
                                    act(o2, in_, AF.Copy, [key_in], [key_out])
                                else:
                                    tt('dve', o2, in_, onesw[:], ALU.mult, [key_in, 'onesw'], [key_out])

                            def group(lh_fn, rh_fn, rkeys):
                                b_ = nbk()
                                for hh, h in enumerate(hs):
                                    mm(banks[b_][0:64, hh * 64:(hh + 1) * 64], lh_fn(hh, h), rh_fn(hh, h), True, True, rkeys, [pk[b_]])
                                return b_

                            RH, AH, BH, KH = 0, 1, 2, 3
                            Lb_, Lpb_, TTb_ = B['Lb'], B['Lpb'], B['TTb']
                            b_ = group(lambda hh, h: fmT(BH, h), lambda hh, h: fmT(AH, h), ['FM'])
                            tt('dve', R_(Lpb_[0][0]).rearrange("p h c -> p (h c)"), banks[b_][0:64, :], cmf[:, 0, :], ALU.mult, [pk[b_], 'cmf'], [Lpb_[0][1]])
                            yield
                            b_ = group(lambda hh, h: fmT(AH, h), lambda hh, h: fmT(BH, h), ['FM'])
                            tt('dve', R_(Lb_[0][0]).rearrange("p h c -> p (h c)"), banks[b_][0:64, :], cmf[:, 2, :], ALU.mult, [pk[b_], 'cmf'], [Lb_[0][1]])
                            tt('dve', R_(TTb_[0][0]).rearrange("p h c -> p (h c)"), Lpb_[0][0].rearrange("p h c -> p (h c)"),
                               irepf[:].rearrange("p h c -> p (h c)"), ALU.add, [Lpb_[0][1], 'irepf'], [TTb_[0][1]])
                            yield
                            if own:
                                b_ = group(lambda hh, h: fmT(BH, h), lambda hh, h: fmT(RH, h), ['FM'])
                                evac(B['MrbT'], banks[b_][0:64, :], pk[b_], mask=MU_I)
                                yield
                            b_ = group(lambda hh, h: fmT(KH, h), lambda hh, h: fmT(AH, h), ['FM'])
                            evac(B['MakT'], banks[b_][0:64, :], pk[b_], mask=MU_S)
                            yield
                            if own:
                                b_ = group(lambda hh, h: fmT(KH, h), lambda hh, h: fmT(RH, h), ['FM'])
                                evac(B['MrkT'], banks[b_][0:64, :], pk[b_], mask=MU_I)
                                yield
                            for it in range(5):
                                ci, ni = it % 2, (it + 1) % 2
                                b1 = group(lambda hh, h: R_(Lpb_[ci][0])[:, hh, :], lambda hh, h: R_(Lb_[ci][0])[:, hh, :], [Lpb_[ci][1], Lb_[ci][1]])
                                evacr(Lb_[ni], banks[b1][0:64, :], pk[b1])
                                yield
                                if it < 4:
                                    b2 = group(lambda hh, h: R_(Lb_[ci][0])[:, hh, :], lambda hh, h: R_(Lpb_[ci][0])[:, hh, :], [Lpb_[ci][1], Lb_[ci][1]])
                                    evacr(Lpb_[ni], banks[b2][0:64, :], pk[b2])
                                    yield
                                b3 = nbk()
                                for hh, h in enumerate(hs):
                                    mm(banks[b3][0:64, hh * 64:(hh + 1) * 64], R_(Lb_[ni][0])[:, hh, :], R_(TTb_[ci][0])[:, hh, :], True, True, [Lb_[ni][1], TTb_[ci][1]], [pk[b3]])
                                tt('dve', R_(TTb_[ni][0]).rearrange("p h c -> p (h c)"), banks[b3][0:64, :], TTb_[ci][0].rearrange("p h c -> p (h c)"),
                                   ALU.add, [pk[b3], TTb_[ci][1]], [TTb_[ni][1]])
                                yield
                            TTf, kTT = B['TTfb']
                            cp('pool', TTf.rearrange("p h c -> p (h c)"), TTb_[1][0].rearrange("p h c -> p (h c)"), [TTb_[1][1]], [kTT])
                            MakT_, MrbT_, MrkT_ = B['MakT'], B['MrbT'], B['MrkT']
                            Wb_, Atb_, Vtb_, RtT_, Pmb_ = B['Wb'], B['Atb'], B['Vtb'], B['RtT'], B['Pmb']
                            b_ = group(lambda hh, h: MakT_[0][:, hh, :], lambda hh, h: tmop("vb", h)[0], [MakT_[1], tmop("vb", 0)[1]])
                            evac(Wb_, banks[b_][0:64, :], pk[b_])
                            yield
                            b_ = group(lambda hh, h: TTf[:, hh, :], lambda hh, h: tmop("ah", h)[0], [kTT, tmop("ah", 0)[1]])
                            evac(Atb_, banks[b_][0:64, :], pk[b_])
                            yield
                            b_ = group(lambda hh, h: TTf[:, hh, :], lambda hh, h: Wb_[0][:, hh, :], [kTT, Wb_[1]])
                            evac(Vtb_, banks[b_][0:64, :], pk[b_])
                            yield
                            if own:
                                b_ = group(lambda hh, h: Atb_[0][:, hh, :], lambda hh, h: MrbT_[0][:, hh, :], [Atb_[1], MrbT_[1]])
                                tt('dve', RtT_[0], banks[b_][0:64, :].rearrange("p (h c) -> p h c", c=64), FM[:, hg * 8:(hg + 1) * 8, RH, cs],
                                   ALU.add, [pk[b_], 'FM'], [RtT_[1]])
                                yield
                                b_ = nbk()
                                for hh, h in enumerate(hs):
                                    o = banks[b_][0:64, hh * 64:(hh + 1) * 64]
                                    mm(o, MrbT_[0][:, hh, :], Vtb_[0][:, hh, :], True, False, [MrbT_[1], Vtb_[1]], [pk[b_]])
                                    mm(o, MrkT_[0][:, hh, :], tmop("vb", h)[0], False, False, [MrkT_[1], tmop("vb", h)[1]], [pk[b_]])
                                    mm(o, RtT_[0][:, hh, :], Zb[:, h, :], False, True, [RtT_[1], 'Zb'], [pk[b_]])
                                if c == 0:
                                    cp('dve', Yt[0:64, hg * 512:(hg + 1) * 512], banks[b_][0:64, :], [pk[b_]], ['Yt'])
                                else:
                                    act(Ych[:, hg * 512:(hg + 1) * 512], banks[b_][0:64, :], AF.Copy, [pk[b_]], ['TMP'])
                                yield
                            b_ = nbk()
                            for hh, h in enumerate(hs):
                                o = banks[b_][0:64, hh * 64:(hh + 1) * 64]
                                mm(o, Atb_[0][:, hh, :], tmop("bb", h)[0], True, True, [Atb_[1], tmop("bb", h)[1]], [pk[b_]])
                            fmsrc = (tm["Fm"][0:64, hg * 512:(hg + 1) * 512], 't_Fm') if c == 0 else (sh["Fm"][:, hg * 512:(hg + 1) * 512], 's_Fm')
                            tt('dve', Pmb_[0].rearrange("p h c -> p (h c)"), banks[b_][0:64, :], fmsrc[0], ALU.add, [pk[b_], fmsrc[1]], [Pmb_[1]])
                            yield
                            b_ = nbk()
                            for hh, h in enumerate(hs):
                                o = banks[b_][0:64, hh * 64:(hh + 1) * 64]
                                mm(o, Pmb_[0][:, hh, :], Zb[:, h, :], True, False, [Pmb_[1], 'Zb'], [pk[b_]])
                                mm(o, tmop("bb", h)[0], Vtb_[0][:, hh, :], False, False, [tmop("bb", h)[1], Vtb_[1]], [pk[b_]])
                                mm(o, tmop("kb", h)[0], tmop("vb", h)[0], False, True, [tmop("kb", h)[1], tmop("vb", h)[1]], [pk[b_]])
                            cp('dve', Zb[:, hg * 8:(hg + 1) * 8, :].rearrange("p h c -> p (h c)"), banks[b_][0:64, :], [pk[b_]], ['Zb'])
                            yield

                        gens = [chunk_steps(0), chunk_steps(1)]
                        live = [True, True]
                        while any(live):
                            for gi_ in range(2):
                                if live[gi_]:
                                    try:
                                        next(gens[gi_])
                                    except StopIteration:
                                        live[gi_] = False
                        if own and c == 1:
                            dma('sp', Yt[64:128, :], Ych, ['TMP'], ['Yt'])
                    if not own:
                        continue
                    S.op('dve', lambda: V_.tensor_reduce(out=st16[:], in_=h3(Yt[:]), axis=AX.X, op=ALU.add), ['Yt'], ['st16'])
                    ts('dve', st16[:], st16[:], 1.0 / 64, None, ALU.mult, None, ['st16'], ['st16'])
                    tt('dve', h3(Yt[:]), h3(Yt[:]), bc16(st16[:]), ALU.subtract, ['Yt', 'st16'], ['Yt'])
                    tt('pool', TMP[:], Yt[:], Yt[:], ALU.mult, ['Yt'], ['TMP'])
                    S.op('dve', lambda: V_.tensor_reduce(out=st16b[:], in_=h3(TMP[:]), axis=AX.X, op=ALU.add), ['TMP'], ['st16b'])
                    ts('dve', st16b[:], st16b[:], 1.0 / 64, GN_EPS, ALU.mult, ALU.add, ['st16b'], ['st16b'])
                    act(st16b[:], st16b[:], AF.Sqrt, ['st16b'], ['st16b'])
                    S.op('dve', lambda: V_.reciprocal(out=st16b[:], in_=st16b[:]), ['st16b'], ['st16b'])
                    tt('dve', h3(Yt[:]), h3(Yt[:]), bc16(st16b[:]), ALU.mult, ['Yt', 'st16b'], ['Yt'])
                    tt('pool', Yt[:], Yt[:], cvec["ln_w"][:], ALU.mult, ['Yt', 'c_ln_w'], ['Yt'])
                    tt('pool', Yt[:], Yt[:], cvec["ln_b"][:], ALU.add, ['Yt', 'c_ln_b'], ['Yt'])
                    tt('dve', TMP[:], Rb[:], A5[:], ALU.mult, ['Rb', 'A5'], ['TMP'])
                    tt('pool', TMP[:], TMP[:], cvec["r_k"][:], ALU.mult, ['TMP', 'c_r_k'], ['TMP'])
                    S.op('dve', lambda: V_.tensor_reduce(out=st16[:], in_=h3(TMP[:]), axis=AX.X, op=ALU.add), ['TMP'], ['st16'])
                    tt('dve', h3(TMP[:]), h3(Vb[:]), bc16(st16[:]), ALU.mult, ['Vb', 'st16'], ['TMP'])
                    tt('pool', Yt[:], Yt[:], TMP[:], ALU.add, ['Yt', 'TMP'], ['Yt'])
                    tt('dve', yb[:], Yt[:], A3[:], ALU.mult, ['Yt', 'A3'], ['yb'])
                    b_ = nbk()
                    pbv = banks[b_][:].bitcast(BF16)
                    for kk in range(8):
                        tr(pbv[:, kk * 128:(kk + 1) * 128], yb[:, kk * 128:(kk + 1) * 128], identb[:], ['yb', 'identb'], [pk[b_]])
                    cp('dve', mst[:].rearrange("p k t -> p (k t)"), pbv, [pk[b_]], ['mst'])
                    t16 = tile - 16
                    dma('sp', mixT_d[8:16, :, t16 * 128:(t16 + 1) * 128].rearrange("k p t -> p k t"), mst[:], ['mst'], ['mixT_d'])
            S.barrier()

        if "dbg_mixT_d" in dbg:
            dma('sp', dbg["dbg_mixT_d"], mixT_d, ['mixT_d'], ['dbg_mixT_d'])
        if "dbg_mixF" in dbg:
            dma('sp', dbg["dbg_mixF"], mixT_d[0:8], ['mixT_d'], ['dbg_mixF'])
        if "dbg_mixR" in dbg:
            dma('sp', dbg["dbg_mixR"], mixT_d[8:16], ['mixT_d'], ['dbg_mixR'])

        if "E" in stages:
            with ExitStack() as ph:
                wo = sb(ph, "wo", [128, KD, D], BF16)
                g1bc = sb(ph, "g1bc", [128, D], F32)
                gtmp = sb(ph, "gtmp", [128, 128], F32)
                mx = [sb(ph, f"mx{i}", [128, KD, 128], BF16) for i in range(2)]
                xo = [sb(ph, f"xo{i}", [128, D], F32) for i in range(2)]
                x1 = [sb(ph, f"x1{i}", [128, D], F32) for i in range(2)]
                junk2 = sb(ph, "junk2", [128, D], BF16)
                xn2 = sb(ph, "xn2", [128, D], BF16)
                ss2 = sb(ph, "ss2", [128, 16], F32)
                rstd2 = sb(ph, "rstd2", [128, 16], F32)
                scl2 = sb(ph, "scl2", [128, KD], F32)
                nw2 = sb(ph, "nw2", [128, KD], F32)
                h2s = [sb(ph, f"h2s{i}", [128, KD, 128], BF16) for i in range(2)]
                wov = I["w_out"].rearrange("(k p) c -> p k c", p=128)
                for q4 in range(4):
                    dma('pool', wo[:, q4 * 4:(q4 + 1) * 4, :], wov[:, q4 * 4:(q4 + 1) * 4, :], [], ['wo'])
                dma('sp', nw2[:], I["nw2T"], [], ['nw2'])
                stt(scl2[:], sc2, 1.0, nw2[:], ALU.add, ALU.mult, ['mod', 'nw2'], ['scl2'])
                for k in range(KD):
                    ts('dve', gtmp[:], ones[:], g1c[:, k:k + 1], None, ALU.mult, None, ['ones', 'mod'], ['gtmp'])
                    mm(banks[0][:, 0:128], gtmp[:], ident[:], True, True, ['gtmp', 'ident'], [pk[0]])
                    cp('dve', g1bc[:, k * 128:(k + 1) * 128], banks[0][:, 0:128], [pk[0]], ['g1bc'])
                mview = mixT_d.rearrange("k p t -> p k t")
                for t16 in range(16):
                    m_ = mx[t16 % 2]
                    mk = f"mx{t16 % 2}"
                    xo_ = xo[t16 % 2]
                    xk = f"xo{t16 % 2}"
                    x1_ = x1[t16 % 2]
                    x1k = f"x1{t16 % 2}"
                    dma('sp', m_[:], mview[:, :, t16 * 128:(t16 + 1) * 128], ['mixT_d'], [mk])
                    dma('sp', xo_[:], I["xc"][OWN + t16 * 128:OWN + (t16 + 1) * 128, :], [], [xk])
                    for nb in range(4):
                        bk = 1 + nb
                        for k in range(KD):
                            mm(banks[bk][:], m_[:, k, :], wo[:, k, nb * 512:(nb + 1) * 512], k == 0, k == KD - 1, [mk, 'wo'], [pk[bk]])
                        tt('dve', x1_[:, nb * 512:(nb + 1) * 512], banks[bk][:], g1bc[:, nb * 512:(nb + 1) * 512], ALU.mult, [pk[bk], 'g1bc'], [x1k])
                        tt('pool', x1_[:, nb * 512:(nb + 1) * 512], x1_[:, nb * 512:(nb + 1) * 512], xo_[:, nb * 512:(nb + 1) * 512], ALU.add, [x1k, xk], [x1k])
                    dma('sp', x1_d[t16 * 128:(t16 + 1) * 128, :], x1_[:], [x1k], ['x1_d'])
                    act(junk2[:], x1_[:], AF.Square, [x1k], ['junk2'], accum_out=ss2[:, t16:t16 + 1])
                    ts('dve', rstd2[:, t16:t16 + 1], ss2[:, t16:t16 + 1], 1.0 / D, NORM_EPS, ALU.mult, ALU.add, ['junk2'], ['rstd2'])
                    act(rstd2[:, t16:t16 + 1], rstd2[:, t16:t16 + 1], AF.Sqrt, ['rstd2'], ['rstd2'])
                    S.op('dve', lambda: V_.reciprocal(out=rstd2[:, t16:t16 + 1], in_=rstd2[:, t16:t16 + 1]), ['rstd2'], ['rstd2'])
                    act(xn2[:], x1_[:], AF.Copy, [x1k, 'rstd2'], ['xn2'], scale=rstd2[:, t16:t16 + 1])
                    hs = h2s[t16 % 2]
                    hk = f"h2s{t16 % 2}"
                    for half in range(2):
                        pbv = banks[5 + half][:].bitcast(BF16)
                        for kk in range(8):
                            k = half * 8 + kk
                            tr(pbv[:, kk * 128:(kk + 1) * 128], xn2[:, k * 128:(k + 1) * 128], identb[:], ['xn2', 'identb'], [pk[5 + half]])
                        for kk in range(8):
                            k = half * 8 + kk
                            if kk % 2 == 0:
                                ts('dve', hs[:, k, :], pbv[:, kk * 128:(kk + 1) * 128], scl2[:, k:k + 1], sh2[:, k:k + 1], ALU.mult, ALU.add,
                                   [pk[5 + half], 'scl2', 'mod'], [hk])
                            else:
                                act(hs[:, k, :], pbv[:, kk * 128:(kk + 1) * 128], AF.Identity, [pk[5 + half], 'scl2', 'mod'], [hk],
                                    scale=scl2[:, k:k + 1], bias=sh2[:, k:k + 1])
                    dma('sp', h2T_d[:, :, t16 * 128:(t16 + 1) * 128], hs[:], [hk], ['h2T_d'])
            S.barrier()
        if "dbg_x1_d" in dbg:
            dma('sp', dbg["dbg_x1_d"], x1_d, ['x1_d'], ['dbg_x1_d'])
        if "dbg_h2T_d" in dbg:
            dma('sp', dbg["dbg_h2T_d"], h2T_d, ['h2T_d'], ['dbg_h2T_d'])

        if "F" in stages:
            with ExitStack() as ph:
                wq = sb(ph, "wq", [128, KD, D], BF16)
                h2b0 = [sb(ph, f"h2b0{i}", [128, KD, 128], BF16) for i in range(2)]
                qT0 = [sb(ph, f"qT0{i}", [128, 16, 128], F32) for i in range(2)]
                wqv = I["w_query"].rearrange("(k p) c -> p k c", p=128)
                for q4 in range(4):
                    dma('pool', wq[:, q4 * 4:(q4 + 1) * 4, :], wqv[:, q4 * 4:(q4 + 1) * 4, :], [], ['wq'])
                for tb in range(16):
                    hb = h2b0[tb % 2]
                    hbk = f"h2b0{tb % 2}"
                    qo = qT0[tb % 2]
                    qk = f"qT0{tb % 2}"
                    dma('sp', hb[:], h2T_d[:, :, tb * 128:(tb + 1) * 128], ['h2T_d'], [hbk])
                    for hp in range(16):
                        bk = (hp // 4) + 4 * (tb % 2)
                        for k in range(KD):
                            mm(banks[bk][:, (hp % 4) * 128:(hp % 4 + 1) * 128], wq[:, k, hp * 128:(hp + 1) * 128], hb[:, k, :],
                               k == 0, k == KD - 1, ['wq', hbk], [pk[bk]])
                    for b4 in range(4):
                        bk = b4 + 4 * (tb % 2)
                        o = qo[:, b4 * 4:(b4 + 1) * 4, :].rearrange("p a t -> p (a t)")
                        if b4 % 2:
                            act(o, banks[bk][:], AF.Copy, [pk[bk]], [qk])
                        else:
                            cp('dve', o, banks[bk][:], [pk[bk]], [qk])
                    dma('sp', qp_d[:, :, tb * 128:(tb + 1) * 128], qo[:], [qk], ['qp_d'])
            S.barrier()

        if "F" in stages:
            with ExitStack() as ph:
                keysT = sb(ph, "keysT", [128, 16, 128], F32)
                iota = sb(ph, "iota", [128, 128], F32)
                bmask = sb(ph, "bmask", [128, 8], F32)
                onec = sb(ph, "onecF", [128, 1], F32)
                g2bc = sb(ph, "g2bc", [128, D], F32)
                gtmp = sb(ph, "gtmp2", [128, 128], F32)
                h2blk = sb(ph, "h2blk", [128, KD, 256], BF16)
                bufA = sb(ph, "bufA", [128, 2048], F32)
                bufB = sb(ph, "bufB", [128, 2048], F32)
                bufC = sb(ph, "bufC", [128, 2048], F32)
                bufD = sb(ph, "bufD", [128, 2048], F32)
                bufE = sb(ph, "bufE", [128, 2048], F32)
                scs = bufA[:].rearrange("p (a t) -> p a t", t=128)
                scr = bufC[:].rearrange("p (a t) -> p a t", t=128)
                cand = bufB[:].rearrange("p (h c) -> p h c", c=256)
                scr2 = bufC[:].rearrange("p (h c) -> p h c", c=256)
                ee = bufD[:].rearrange("p (h c) -> p h c", c=256)
                qTs = bufE[:].rearrange("p (a t) -> p a t", t=128)
                gT = bufE[:].rearrange("p (a t) -> p a t", t=128)
                top = sb(ph, "top", [128, 16, 16], F32)
                idx = sb(ph, "idx", [128, 16, 16], U32)
                idxf0 = sb(ph, "idxf0", [128, 8, 16], F32)
                idxf1 = sb(ph, "idxf1", [128, 8, 16], F32)
                c8a = sb(ph, "c8a", [128, 8, 8], F32)
                c8b = sb(ph, "c8b", [128, 8, 8], F32)
                zz = sb(ph, "zz", [128, 8], F32)
                gateP = sb(ph, "gateP", [128, 16, 8, 16], F32)
                JTs = sb(ph, "JTs", [128, 128], F32)
                dJ1 = sb(ph, "dJ0", [128, 16, 128], BF16)
                dJ = [dJ1, dJ1]
                OHI = [sb(ph, f"OHI{i}", [128, 16, 128], BF16) for i in range(2)]
                OHJ = [sb(ph, f"OHJ{i}", [128, 16, 128], BF16) for i in range(2)]
                GTb = [sb(ph, f"GTb{i}", [128, 16, 8, 16], BF16) for i in range(2)]
                Bs = [sb(ph, f"Bs{i}", [128, 16, 128], BF16) for i in range(2)]
                Gsb = sb(ph, "Gsb", [128, 128, 256], BF16)
                NB_ = 4
                uTt = [sb(ph, f"uTt{i}", [128, KD, 128], BF16) for i in range(NB_)]
                vh = [sb(ph, f"vh{i}", [128, 1024], BF16) for i in range(NB_)]
                gel = [sb(ph, f"gel{i}", [128, 256], F32) for i in range(2)]
                dma('sp', keysT[:], I["keysT"], [], ['keysT'])
                dma('sp', iota[:], I["iota"], [], ['iota'])
                dma('sp', bmask[:], I["blockmask"], [], ['bmask'])
                S.op('dve', lambda: V_.memset(onec[:], 1.0), [], ['onecF'])
                for k in range(KD):
                    ts('dve', gtmp[:], ones[:], g2c[:, k:k + 1], None, ALU.mult, None, ['ones', 'mod'], ['gtmp2'])
                    mm(banks[0][:, 0:128], gtmp[:], ident[:], True, True, ['gtmp2', 'ident'], [pk[0]])
                    cp('dve', g2bc[:, k * 128:(k + 1) * 128], banks[0][:, 0:128], [pk[0]], ['g2bc'])
                top4 = top[:].rearrange("p (h two) a -> p h two a", two=2)
                idx4 = idx[:].rearrange("p (h two) a -> p h two a", two=2)
                cand4 = cand.rearrange("p h (a b) -> p h a b", b=16)
                ee4 = ee.rearrange("p h (a b) -> p h a b", b=16)
                nonlocal_nld = [0]
                nsub = 0
                for tb in range(8):
                    dma('sp', h2blk[:], h2T_d[:, :, tb * 256:(tb + 1) * 256], ['h2T_d'], ['h2blk'])
                    for half in range(2):
                        tk0 = tb * 256 + half * 128
                        dma('sp', qTs, qp_d[:, :, tk0:tk0 + 128], ['qp_d'], ['bufE'])
                        for hp in range(16):
                            bk = hp // 4
                            mm(banks[bk][:, (hp % 4) * 128:(hp % 4 + 1) * 128], qTs[:, hp, :], keysT[:, hp, :], True, True, ['bufE', 'keysT'], [pk[bk]])
                        for bk in range(4):
                            o = scs[:, bk * 4:(bk + 1) * 4, :].rearrange("p a t -> p (a t)")
                            if bk % 2:
                                act(o, banks[bk][:], AF.Copy, [pk[bk]], ['bufA'])
                            else:
                                cp('dve', o, banks[bk][:], [pk[bk]], ['bufA'])
                        for hp in range(16):
                            S.op('dve', lambda: V_.max(out=top[:, hp, 0:8], in_=scs[:, hp, :]), ['bufA'], ['top'])
                            S.op('dve', lambda: V_.match_replace(out=scr[:, hp, :], in_to_replace=top[:, hp, 0:8], in_values=scs[:, hp, :], imm_value=-1e30),
                                 ['bufA', 'top'], ['bufC'])
                            S.op('dve', lambda: V_.max(out=top[:, hp, 8:16], in_=scr[:, hp, :]), ['bufC'], ['top'])
                            S.op('dve', lambda: V_.max_index(out=idx[:, hp, 0:8], in_max=top[:, hp, 0:8], in_values=scs[:, hp, :]), ['bufA', 'top'], ['idx'])
                            S.op('dve', lambda: V_.max_index(out=idx[:, hp, 8:16], in_max=top[:, hp, 8:16], in_values=scs[:, hp, :]), ['bufA', 'top'], ['idx'])
                        cp('dve', idxf0[:], idx4[:, :, 0, :], ['idx'], ['idxf0'])
                        cp('dve', idxf1[:], idx4[:, :, 1, :], ['idx'], ['idxf1'])
                        tt('dve', cand4, top4[:, :, 0, :].unsqueeze(3).to_broadcast([128, 8, 16, 16]),
                           top4[:, :, 1, :].unsqueeze(2).to_broadcast([128, 8, 16, 16]), ALU.add, ['top'], ['bufB'])
                        for h in range(8):
                            S.op('dve', lambda: V_.max(out=c8a[:, h, :], in_=cand[:, h, :]), ['bufB'], ['c8a'])
                            S.op('dve', lambda: V_.match_replace(out=scr2[:, h, :], in_to_replace=c8a[:, h, :], in_values=cand[:, h, :], imm_value=-1e30),
                                 ['bufB', 'c8a'], ['bufC'])
                            S.op('dve', lambda: V_.max(out=c8b[:, h, :], in_=scr2[:, h, :]), ['bufC'], ['c8b'])
                        tt('dve', ee, cand, c8a[:, :, 0:1].to_broadcast([128, 8, 256]), ALU.subtract, ['bufB', 'c8a'], ['bufD'])
                        act(ee, ee, AF.Exp, ['bufD'], ['bufD'])
                        tt('dve', scr2, cand, c8b[:, :, 7:8].to_broadcast([128, 8, 256]), ALU.is_ge, ['bufB', 'c8b'], ['bufC'])
                        tt('dve', ee, ee, scr2, ALU.mult, ['bufD', 'bufC'], ['bufD'])
                        S.op('dve', lambda: V_.tensor_reduce(out=zz[:], in_=ee, axis=AX.X, op=ALU.add), ['bufD'], ['zz'])
                        S.op('dve', lambda: V_.reciprocal(out=zz[:], in_=zz[:]), ['zz'], ['zz'])
                        tt('dve', gateP[:].rearrange("p a h b -> p h a b"), ee4, zz[:].unsqueeze(2).unsqueeze(3).to_broadcast([128, 8, 16, 16]),
                           ALU.mult, ['bufD', 'zz'], ['gateP'])
                        tr(banks[0][:, 0:128], idxf0[:].rearrange("p h a -> p (h a)"), ident[:], ['idxf0', 'ident'], [pk[0]])
                        tr(banks[0][:, 128:256], idxf1[:].rearrange("p h a -> p (h a)"), ident[:], ['idxf1', 'ident'], [pk[0]])
                        act(JTs[:], banks[0][:, 128:256], AF.Copy, [pk[0]], ['JTs'])
                        for a in range(16):
                            bk = 4 + a // 4
                            tr(banks[bk][:, (a % 4) * 128:(a % 4 + 1) * 128], gateP[:, a, :, :].rearrange("p h b -> p (h b)"), ident[:],
                               ['gateP', 'ident'], [pk[bk]])
                        for b4 in range(4):
                            o = gT[:, b4 * 4:(b4 + 1) * 4, :].rearrange("p a t -> p (a t)")
                            if b4 % 2:
                                act(o, banks[4 + b4][:], AF.Copy, [pk[4 + b4]], ['bufE'])
                            else:
                                cp('dve', o, banks[4 + b4][:], [pk[4 + b4]], ['bufE'])
                        for sub in range(8):
                            t0 = sub * 16
                            si = nsub % 2
                            nsub += 1
                            tt('pool', GTb[si][:], gT[:, :, t0:t0 + 16].rearrange("p a t -> p t a").unsqueeze(2).to_broadcast([128, 16, 8, 16]),
                               bmask[:].unsqueeze(1).unsqueeze(3).to_broadcast([128, 16, 8, 16]), ALU.mult, ['bufE', 'bmask'], [f'GTb{si}'])
                            tt('dve', OHI[si][:], iota[:].unsqueeze(1).to_broadcast([128, 16, 128]),
                               banks[0][:, t0:t0 + 16].unsqueeze(2).to_broadcast([128, 16, 128]), ALU.is_equal, ['iota', pk[0]], [f'OHI{si}'])
                            tt('pool', dJ[si][:], iota[:].unsqueeze(1).to_broadcast([128, 16, 128]),
                               JTs[:, t0:t0 + 16].unsqueeze(2).to_broadcast([128, 16, 128]), ALU.subtract, ['iota', 'JTs'], ['dJ0'])
                            act(dJ[si][:], dJ[si][:], AF.Square, ['dJ0'], ['dJ0'])
                            act(OHJ[si][:], dJ[si][:], AF.Relu, ['dJ0', 'onecF'], [f'OHJ{si}'], bias=onec[:], scale=-1.0)
                            for t4 in range(4):
                                bk = 2 + t4 % 2
                                for tl in range(4):
                                    t = t4 * 4 + tl
                                    mm(banks[bk][:, tl * 128:(tl + 1) * 128], GTb[si][:, t, :, :].rearrange("p h a -> p (h a)"), OHJ[si][:, t, :], True, True,
                                       [f'GTb{si}', f'OHJ{si}'], [pk[bk]])
                                o = Bs[si][:, t4 * 4:(t4 + 1) * 4, :].rearrange("p t j -> p (t j)")
                                act(o, banks[bk][:], AF.Copy, [pk[bk]], [f'Bs{si}'])
                            for t4 in range(4):
                                bk = 4 + t4
                                for tl in range(4):
                                    t = t4 * 4 + tl
                                    mm(banks[bk][:, tl * 128:(tl + 1) * 128], Bs[si][:, t, :], OHI[si][:, t, :], True, True, [f'Bs{si}', f'OHI{si}'], [pk[bk]])
                                c0 = half * 128 + t0 + t4 * 4
                                o = Gsb[:, :, c0:c0 + 4]
                                src = banks[bk][:].rearrange("p (t i) -> p i t", i=128)
                                if t4 % 4 != 3:
                                    act(o, src, AF.Copy, [pk[bk]], ['Gsb'])
                                else:
                                    cp('dve', o, src, [pk[bk]], ['Gsb'])
                    for dpass in range(2):
                        dc = slice(dpass * 1024, (dpass + 1) * 1024)
                        slot = {}

                        def sw_load(i0):
                            nonlocal_nld[0] += 1
                            bi = nonlocal_nld[0] % NB_
                            slot[i0] = bi
                            dma('sp', vh[bi][:], vb_d[i0 * 128:(i0 + 1) * 128, dc], ['vb_d'], [f"vh{bi}"])
                            if dpass == 0:
                                ut = uTt[bi]
                                utk = f"uTt{bi}"
                                dma('sp', ut[:].rearrange("p k j -> p (k j)"), uTb_d[i0], ['uTb_d'], [utk])
                                bk = 2 + i0 % 2
                                for k in range(KD):
                                    mm(banks[bk][:, 0:256], ut[:, k, :], h2blk[:, k, :], k == 0, k == KD - 1, [utk, 'h2blk'], [pk[bk]])

                        def sw_ga(i0):
                            if dpass == 0:
                                bk = 2 + i0 % 2
                                ge = gel[i0 % 2]
                                gek = f"gel{i0 % 2}"
                                act(ge[:], banks[bk][:, 0:256], AF.Gelu, [pk[bk]], [gek])
                                tt('dve', Gsb[:, i0, :], ge[:], Gsb[:, i0, :], ALU.mult, [gek, 'Gsb'], ['Gsb'])

                        def sw_out(i0):
                            bi = slot[i0]
                            for t2 in range(2):
                                for db in range(2):
                                    ob_ = 4 + t2 * 2 + db
                                    mm(banks[ob_][:], Gsb[:, i0, t2 * 128:(t2 + 1) * 128], vh[bi][:, db * 512:(db + 1) * 512], i0 == 0, i0 == 127,
                                       ['Gsb', f"vh{bi}"], [pk[ob_]])

                        sw_load(0)
                        for i0 in range(128):
                            sw_ga(i0)
                            if i0 + 1 < 128:
                                sw_load(i0 + 1)
                            sw_out(i0)
                        for t2 in range(2):
                            r0 = tb * 256 + t2 * 128
                            yo_ = bufA[:, t2 * 1024:(t2 + 1) * 1024]
                            x1_ = bufB[:, t2 * 1024:(t2 + 1) * 1024]
                            dma('sp', x1_, x1_d[r0:r0 + 128, dc], ['x1_d'], ['bufB'])
                            for db in range(2):
                                ob_ = 4 + t2 * 2 + db
                                tt('dve', yo_[:, db * 512:(db + 1) * 512], banks[ob_][:], g2bc[:, dpass * 1024 + db * 512:dpass * 1024 + (db + 1) * 512],
                                   ALU.mult, [pk[ob_], 'g2bc'], ['bufA'])
                            tt('pool', yo_, yo_, x1_, ALU.add, ['bufA', 'bufB'], ['bufA'])
                            dma('sp', y[r0:r0 + 128, dc], yo_, ['bufA'], ['y'])
            S.barrier()

        S.barrier()
        if "Y0" in stages:
            with ExitStack() as ph:
                z = sb(ph, "zout", [128, D], F32)
                S.op('dve', lambda: V_.memset(z[:], 0.0), [], ['zout'])
                for i in range(16):
                    dma('sp', y[i * 128:(i + 1) * 128, :], z[:], ['zout'], ['y'])
        S.finish()
    return nc


def host_inputs(inputs):
    g = {k: np.asarray(v) for k, v in inputs.items()}
    f32 = np.float32
    x = g["x"]
    L = 0
    common = {
        "w_ada": np.ascontiguousarray(g["w_ada"][L]),
        "b_adaT": np.ascontiguousarray(g["b_ada"][L].reshape(96, 128).T),
        "nw1T": np.ascontiguousarray(g["norm_mix_w"][L].reshape(KD, 128).T),
        "w_in": np.ascontiguousarray(g["w_in"][L]),
        "qnw": np.ascontiguousarray(g["fox_q_norm_w"][L].reshape(128, 1)),
        "knw": np.ascontiguousarray(g["fox_k_norm_w"][L].reshape(128, 1)),
        "fbias": np.ascontiguousarray(g["fox_f_bias"][L]),
        "mu": np.ascontiguousarray(g["rwkv_mu"][L]),
        "w0": np.ascontiguousarray(g["rwkv_w0"][L]),
        "w_up": np.ascontiguousarray(g["rwkv_w_up"][L]),
        "a0": np.ascontiguousarray(g["rwkv_a0"][L]),
        "a_up": np.ascontiguousarray(g["rwkv_a_up"][L]),
        "g_up": np.ascontiguousarray(g["rwkv_g_up"][L]),
        "k_k": np.ascontiguousarray(g["rwkv_k_k"][L]),
        "k_a": np.ascontiguousarray(g["rwkv_k_a"][L]),
        "r_k": np.ascontiguousarray(g["rwkv_r_k"][L].reshape(1024)),
        "ln_w": np.ascontiguousarray(g["rwkv_ln_w"][L]),
        "ln_b": np.ascontiguousarray(g["rwkv_ln_b"][L]),
        "w_out": np.ascontiguousarray(g["w_out"][L]),
        "nw2T": np.ascontiguousarray(g["norm_ffn_w"][L].reshape(KD, 128).T),
        "w_query": np.ascontiguousarray(g["peer_w_query"][L]),
        "keysT": np.ascontiguousarray(g["peer_sub_keys"][L].reshape(16, 128, 128).transpose(2, 0, 1)),
        "uT": np.ascontiguousarray(g["peer_u"][L].reshape(128, 128, KD, 128).transpose(0, 3, 2, 1)),
        "v": np.ascontiguousarray(g["peer_v"][L]),
    }
    ar = np.arange(128)
    ident = np.eye(128, dtype=f32)
    tri = (ar[:, None] <= ar[None, :]).astype(f32)
    same = (ar[:, None] // 64 == ar[None, :] // 64)
    a64 = np.arange(64)
    mu_strict = (a64[None, :] > a64[:, None]).astype(f32)
    mu_incl = (a64[None, :] >= a64[:, None]).astype(f32)
    ml_strict = (a64[None, :] < a64[:, None]).astype(f32)
    cmask = np.stack([np.tile(m, (1, 8)) for m in (mu_strict, mu_incl, ml_strict)], axis=1)
    common.update({
        "ident": ident, "tri": tri, "ones": np.ones((128, 128), f32),
        "e0sel": (ar[:, None] == 0).astype(f32) * np.ones((1, 128), f32),
        "tri2": (tri * same).astype(f32), "blk2": same.astype(f32),
        "cmask": np.ascontiguousarray(cmask.astype(f32)),
        "idpat": ((ar[:, None] % 64) == a64[None, :]).astype(f32),
        "iota": np.tile(ar[None, :].astype(f32), (128, 1)),
        "blockmask": ((ar[:, None] // 16) == np.arange(8)[None, :]).astype(f32),
    })
    maps = []
    for core in range(8):
        b, s = core // 2, core % 2
        if s == 1:
            xc = np.ascontiguousarray(x[b])
        else:
            xc = np.concatenate([x[b, :OWN], x[b, :OWN]], axis=0)
        pf = np.ones((128, 1), f32)
        pf[0, 0] = float(s)
        m = dict(common)
        m.update({
            "xc": xc, "cT": np.ascontiguousarray(g["c"][b].reshape(KD, 128).T),
            "flagcol": np.full((128, 1), float(s), f32), "pflag": pf,
        })
        maps.append(m)
    return maps


def kernel(**inputs):
    nc = build()
    maps = host_inputs(inputs)
    maps = [{k: m[k] for k in nc._used_inputs} for m in maps]
    res = run_bass_kernel_spmd(nc, maps, core_ids=list(range(8)))
    out = np.zeros((4, 4096, D), np.float32)
    for core in range(8):
        b, s = core // 2, core % 2
        out[b, s * OWN:(s + 1) * OWN] = res.results[core]["y"]
    return out
```
